# Optimizing a Trainium2 kernel written in Bass

```python
import math
import jax
import jax.numpy as jnp
from jax import lax
import numpy as np


D_MODEL = 1024
BATCH = 4
SEQ = 8192
DEPTH = 2

N_META = 16
POOL_GROUPS = 4
POOL_WINDOWS = (2, 4, 8, 16)
POOL_WIDTH = D_MODEL // 2
POOL_GROUP_DIM = POOL_WIDTH // POOL_GROUPS
DIFF_HEADS = 4
DIFF_QK_DIM = D_MODEL // 16
DIFF_V_DIM = 2 * DIFF_QK_DIM
Q_WIDTH = DIFF_HEADS * 2 * DIFF_QK_DIM
ATTN_WIDTH = DIFF_HEADS * DIFF_V_DIM
N_BRANCHES = 2
IN_COLS = POOL_WIDTH + 2 * Q_WIDTH + ATTN_WIDTH + N_BRANCHES * D_MODEL
REL_BUCKETS = 32
REL_MAX_DIST = 128
Q_BLOCK = 128
D_FF = 7 * D_MODEL // 2
N_EXPERTS = 8
TOP_K = 2
N_DENSE = (DEPTH + 1) // 2
N_MOE = DEPTH // 2
RMS_EPS = 1e-6
SUBLN_EPS = 1e-5

kernel_name = 'hybrid_pool_diffattn_moe_block'


def rmsnorm(x, g, eps=RMS_EPS):
    xf = x.astype(jnp.float32)
    y = xf * lax.rsqrt(jnp.mean(xf * xf, axis=-1, keepdims=True) + eps)
    return (y * g.astype(jnp.float32)).astype(x.dtype)


def t5_causal_bucket(q_pos, k_pos):
    n = jnp.maximum(q_pos[:, None] - k_pos[None, :], 0)
    max_exact = REL_BUCKETS // 2
    nf = jnp.maximum(n, 1).astype(jnp.float32)
    large = max_exact + (jnp.log(nf / max_exact) / math.log(REL_MAX_DIST / max_exact)
                         * (REL_BUCKETS - max_exact)).astype(jnp.int32)
    large = jnp.minimum(large, REL_BUCKETS - 1)
    return jnp.where(n < max_exact, n, large)


def pool_mixer(u, group_w, scale):
    b, L, _ = u.shape
    uf = u.astype(jnp.float32).reshape(b, L, POOL_GROUPS, POOL_GROUP_DIM)
    cs = jnp.pad(jnp.cumsum(uf, axis=1), ((0, 0), (1, 0), (0, 0), (0, 0)))
    t = jnp.arange(L)
    avgs = []
    for g, w in enumerate(POOL_WINDOWS):
        csg = cs[:, :, g]
        prev = jnp.pad(csg, ((0, 0), (w - 1, 0), (0, 0)))[:, :L]
        cnt = jnp.minimum(t + 1, w).astype(jnp.float32)[None, :, None]
        avgs.append((csg[:, 1:] - prev) / cnt)
    mixed = (jnp.stack(avgs, axis=2) - uf).astype(u.dtype)
    y = jnp.einsum('blgc,gcd->blgd', mixed, group_w)
    return y.reshape(b, L, POOL_WIDTH) * scale


def diff_attention(q, k, v, rel_bias, lam, lambda_init, subln_gain):
    b, L, H = q.shape[0], q.shape[1], q.shape[2]
    n_blocks = -(-L // Q_BLOCK)
    Lp = n_blocks * Q_BLOCK
    pad5 = ((0, 0), (0, Lp - L), (0, 0), (0, 0), (0, 0))
    q = jnp.pad(q, pad5) * (DIFF_QK_DIM ** -0.5)
    k = jnp.pad(k, pad5)
    v = jnp.pad(v, pad5[:4])
    k_pos = jnp.arange(Lp)
    q_blocks = q.reshape(b, n_blocks, Q_BLOCK, H, 2, DIFF_QK_DIM).transpose(1, 0, 2, 3, 4, 5)
    starts = jnp.arange(n_blocks) * Q_BLOCK

    def one_block(args):
        qb, s = args
        q_pos = s + jnp.arange(Q_BLOCK)
        bias = rel_bias[t5_causal_bucket(q_pos, k_pos)]
        bias = bias.transpose(2, 0, 1).astype(jnp.float32)
        logits = jnp.einsum('bqhcd,bkhcd->bhcqk', qb, k).astype(jnp.float32)
        logits = logits + bias[None, :, None]
        causal = k_pos[None, :] <= q_pos[:, None]
        logits = jnp.where(causal, logits, -jnp.inf)
        p = jax.nn.softmax(logits, axis=-1)
        a = p[:, :, 0] - lam * p[:, :, 1]
        return jnp.einsum('bhqk,bkhd->bqhd', a.astype(v.dtype), v)

    o = lax.map(one_block, (q_blocks, starts))
    o = o.transpose(1, 0, 2, 3, 4).reshape(b, Lp, H, DIFF_V_DIM)[:, :L]
    o = rmsnorm(o, subln_gain, SUBLN_EPS) * (1.0 - lambda_init)
    return o.reshape(b, L, H * DIFF_V_DIM)


def swiglu(h, wg, wu, wd):
    return (jax.nn.silu(h @ wg) * (h @ wu)) @ wd


def moe_swiglu(h, router, wg, wu, wd):
    logits = jnp.einsum('bld,de->ble', h, router).astype(jnp.float32)
    top_vals, top_idx = lax.top_k(logits, TOP_K)
    top_w = jax.nn.softmax(top_vals, axis=-1)
    combine = jnp.sum(jax.nn.one_hot(top_idx, N_EXPERTS, dtype=jnp.float32) * top_w[..., None], axis=-2)
    combine = combine.astype(h.dtype)
    out = jnp.zeros_like(h)
    for e in range(N_EXPERTS):
        out = out + combine[..., e:e + 1] * swiglu(h, wg[e], wu[e], wd[e])
    return out


def setup_inputs(seed: int = 0) -> dict:
    key = jax.random.key(seed)
    ks = jax.random.split(key, 24)
    f32 = jnp.float32

    def nrm(k, shape, scale):
        return jax.random.normal(k, shape, f32) * scale

    def gain(k, shape):
        return 1.0 + 0.02 * jax.random.normal(k, shape, f32)

    return {
        'x': nrm(ks[0], (BATCH, SEQ, D_MODEL), 1.0),
        'meta_tokens': nrm(ks[1], (N_META, D_MODEL), 1.0),
        'rel_bias': nrm(ks[2], (REL_BUCKETS, DIFF_HEADS), 0.5),
        'norm_mix': gain(ks[3], (DEPTH, D_MODEL)),
        'w_in': nrm(ks[4], (DEPTH, D_MODEL, IN_COLS), D_MODEL ** -0.5),
        'pool_group_w': nrm(ks[5], (DEPTH, POOL_GROUPS, POOL_GROUP_DIM, POOL_GROUP_DIM), POOL_GROUP_DIM ** -0.5),
        'pool_scale': 1.0 + 0.1 * jax.random.normal(ks[6], (DEPTH, POOL_WIDTH), f32),
        'lambda_q1': nrm(ks[7], (DEPTH, DIFF_QK_DIM), 0.1),
        'lambda_k1': nrm(ks[8], (DEPTH, DIFF_QK_DIM), 0.1),
        'lambda_q2': nrm(ks[9], (DEPTH, DIFF_QK_DIM), 0.1),
        'lambda_k2': nrm(ks[10], (DEPTH, DIFF_QK_DIM), 0.1),
        'subln_gain': gain(ks[11], (DEPTH, DIFF_V_DIM)),
        'w_pool_up': nrm(ks[12], (DEPTH, POOL_WIDTH, D_MODEL), POOL_WIDTH ** -0.5),
        'w_attn_up': nrm(ks[13], (DEPTH, ATTN_WIDTH, D_MODEL), ATTN_WIDTH ** -0.5),
        'w_out': nrm(ks[14], (DEPTH, D_MODEL, D_MODEL), D_MODEL ** -0.5),
        'norm_ffn': gain(ks[15], (DEPTH, D_MODEL)),
        'dense_w_gate': nrm(ks[16], (N_DENSE, D_MODEL, D_FF), D_MODEL ** -0.5),
        'dense_w_up': nrm(ks[17], (N_DENSE, D_MODEL, D_FF), D_MODEL ** -0.5),
        'dense_w_down': nrm(ks[18], (N_DENSE, D_FF, D_MODEL), D_FF ** -0.5),
        'moe_router': nrm(ks[19], (N_MOE, D_MODEL, N_EXPERTS), D_MODEL ** -0.5),
        'moe_w_gate': nrm(ks[20], (N_MOE, N_EXPERTS, D_MODEL, D_FF), D_MODEL ** -0.5),
        'moe_w_up': nrm(ks[21], (N_MOE, N_EXPERTS, D_MODEL, D_FF), D_MODEL ** -0.5),
        'moe_w_down': nrm(ks[22], (N_MOE, N_EXPERTS, D_FF, D_MODEL), D_FF ** -0.5),
        'final_norm': gain(ks[23], (D_MODEL,)),
    }


def reference(x, meta_tokens, rel_bias, norm_mix, w_in, pool_group_w, pool_scale,
              lambda_q1, lambda_k1, lambda_q2, lambda_k2, subln_gain,
              w_pool_up, w_attn_up, w_out, norm_ffn,
              dense_w_gate, dense_w_up, dense_w_down,
              moe_router, moe_w_gate, moe_w_up, moe_w_down, final_norm):
    b = x.shape[0]
    f32 = jnp.float32
    meta = jnp.broadcast_to(meta_tokens[None].astype(x.dtype), (b, N_META, D_MODEL))
    h = jnp.concatenate([meta, x], axis=1)
    L = h.shape[1]
    splits = [POOL_WIDTH, POOL_WIDTH + Q_WIDTH, POOL_WIDTH + 2 * Q_WIDTH,
              POOL_WIDTH + 2 * Q_WIDTH + ATTN_WIDTH]
    for layer in range(DEPTH):
        hn = rmsnorm(h, norm_mix[layer])
        proj = hn @ w_in[layer]
        u_pool, q, k, v, gate_logits = jnp.split(proj, splits, axis=-1)
        a_out = pool_mixer(u_pool, pool_group_w[layer], pool_scale[layer])
        lambda_init = 0.8 - 0.6 * math.exp(-0.3 * layer)
        lam = (jnp.exp(jnp.sum(lambda_q1[layer].astype(f32) * lambda_k1[layer].astype(f32)))
               - jnp.exp(jnp.sum(lambda_q2[layer].astype(f32) * lambda_k2[layer].astype(f32)))
               + lambda_init)
        q = q.reshape(b, L, DIFF_HEADS, 2, DIFF_QK_DIM)
        k = k.reshape(b, L, DIFF_HEADS, 2, DIFF_QK_DIM)
        v = v.reshape(b, L, DIFF_HEADS, DIFF_V_DIM)
        b_out = diff_attention(q, k, v, rel_bias, lam, lambda_init, subln_gain[layer]).astype(h.dtype)
        gates = jax.nn.sigmoid(gate_logits.astype(f32)).astype(h.dtype).reshape(b, L, N_BRANCHES, D_MODEL)
        merged = (gates[:, :, 0] * (a_out @ w_pool_up[layer])
                  + gates[:, :, 1] * (b_out @ w_attn_up[layer]))
        h = h + merged @ w_out[layer]
        hn = rmsnorm(h, norm_ffn[layer])
        j = layer // 2
        if layer % 2 == 0:
            f = swiglu(hn, dense_w_gate[j], dense_w_up[j], dense_w_down[j])
        else:
            f = moe_swiglu(hn, moe_router[j], moe_w_gate[j], moe_w_up[j], moe_w_down[j])
        h = h + f
    out = rmsnorm(h, final_norm)
    return out[:, N_META:]
```

```python
import math
import numpy as np
import concourse.bass as bass
import concourse.mybir as mybir
from concourse.bass_utils import run_bass_kernel_spmd
from contextlib import ExitStack

F32 = mybir.dt.float32
BF16 = mybir.dt.bfloat16
AF = mybir.ActivationFunctionType
ALU = mybir.AluOpType
AX = mybir.AxisListType

ENGS = ("pe", "act", "dve", "pool", "sp")
NDMA_SEMS = 12
SELF_SYNC = ("act", "dve", "pool")


class Prog:
    def __init__(self, nc):
        self.nc = nc
        self.ops = []
        self.last_w = {}
        self.readers = {}
        self.es = ExitStack()

    _n = [0]
    G = None

    def sbuf(self, name, shape, dt):
        Prog._n[0] += 1
        return self.es.enter_context(self.nc.sbuf_tensor("%s_u%d" % (name, Prog._n[0]), shape, dt))

    def psum(self, name, shape, dt):
        Prog._n[0] += 1
        return self.es.enter_context(self.nc.psum_tensor("%s_u%d" % (name, Prog._n[0]), shape, dt))

    def add(self, eng, fn, reads=(), writes=(), dma=False, partial=False):
        idx = len(self.ops)
        deps = set()
        for r in reads:
            for w in self.last_w.get(r, ()):
                deps.add(w)
        for w in writes:
            for pw in self.last_w.get(w, ()):
                if not (partial and self.ops[pw]["partial"]):
                    deps.add(pw)
            for rd in self.readers.get(w, ()):
                deps.add(rd)
        deps.discard(idx)
        self.ops.append(dict(eng=eng, fn=fn, deps=deps, dma=dma, idx=idx, partial=partial))
        for r in reads:
            self.readers.setdefault(r, []).append(idx)
        for w in writes:
            if partial and not self.readers.get(w):
                self.last_w.setdefault(w, []).append(idx)
            else:
                self.last_w[w] = [idx]
            self.readers[w] = []
        return idx

    def dma(self, q, out, in_, reads=(), writes=(), partial=False, **kw):
        return self.add(q, lambda e: e.dma_start(out=out, in_=in_, **kw), reads, writes, dma=True, partial=partial)

    def emit(self):
        nc = self.nc
        ops = self.ops
        need = [False] * len(ops)
        for o in ops:
            for d in o["deps"]:
                po = ops[d]
                if po["dma"] or po["eng"] != o["eng"] or po["eng"] in SELF_SYNC:
                    need[d] = True
        dmaq = set(o["eng"] for o in ops if o["dma"])
        if Prog.G is None or Prog.G["nc"] is not nc:
            ges = ExitStack()
            Prog.G = dict(
                nc=nc, es=ges,
                esem={e: ges.enter_context(nc.semaphore("gs_" + e)) for e in ENGS},
                dsem={e: [ges.enter_context(nc.semaphore("gd_%s%d" % (e, i))) for i in range(NDMA_SEMS)] for e in ("sp",)},
                ecount={e: 0 for e in ENGS},
                dcount={e: [0] * NDMA_SEMS for e in ENGS},
                dn={e: 0 for e in ENGS})
        G = Prog.G
        esem, dsem, ecount, dcount, dn = G["esem"], G["dsem"], G["ecount"], G["dcount"], G["dn"]
        for o in ops:
            i = o["idx"]
            e = o["eng"]
            o["sig"] = None
            o["prewait"] = None
            if o["dma"]:
                k = dn[e] % NDMA_SEMS
                dn[e] += 1
                if dcount[e][k] > 0:
                    o["prewait"] = (dsem[e][k], dcount[e][k], ("d", e, k))
                dcount[e][k] += 16
                o["sig"] = (dsem[e][k], dcount[e][k], ("d", e, k), 16)
            elif need[i]:
                ecount[e] += 1
                o["sig"] = (esem[e], ecount[e], ("e", e), 1)
        streams = {e: [o for o in ops if o["eng"] == e] for e in ENGS}
        final_waits = {e: [] for e in ENGS}
        for e in dmaq:
            for k in range(NDMA_SEMS):
                if dcount[e][k] > 0:
                    final_waits[e].append((dsem[e][k], dcount[e][k]))

        def run_stream(e, eng):
            waited = {}
            for o in streams[e]:
                ws = []
                if o["prewait"] is not None:
                    ws.append(o["prewait"])
                for d in sorted(o["deps"]):
                    po = ops[d]
                    if po["dma"] or po["eng"] != e or e in SELF_SYNC:
                        s = po["sig"]
                        ws.append((s[0], s[1], s[2]))
                best = {}
                for (sem, val, key) in ws:
                    if key not in best or best[key][1] < val:
                        best[key] = (sem, val)
                for key, (sem, val) in best.items():
                    if waited.get(key, 0) >= val:
                        continue
                    eng.wait_ge(sem, val)
                    waited[key] = val
                ins = o["fn"](eng)
                if o["sig"] is not None:
                    ins.then_inc(o["sig"][0], o["sig"][3])
            for (sem, val) in final_waits[e]:
                eng.wait_ge(sem, val)

        with nc.Block() as block:
            @block.tensor
            def _(eng):
                run_stream("pe", eng)

            @block.scalar
            def _(eng):
                run_stream("act", eng)

            @block.vector
            def _(eng):
                run_stream("dve", eng)

            @block.gpsimd
            def _(eng):
                run_stream("pool", eng)

            @block.sync
            def _(eng):
                run_stream("sp", eng)
        self.es.close()


D = 1024
DFF = 3584
NEXP = 8
LAMBDA_INIT = [0.8 - 0.6 * math.exp(-0.3 * l) for l in range(2)]
NEG = -30000.0


def _bucket(n):
    n = np.maximum(n, 0)
    nf = np.maximum(n, 1).astype(np.float32)
    large = 16 + (np.log(nf / np.float32(16)) / np.float32(math.log(8.0)) * np.float32(16)).astype(np.int32)
    large = np.minimum(large, 31)
    return np.where(n < 16, n, large)


def _const_tables():
    k = np.arange(128)[:, None]
    q = np.arange(128)[None, :]
    oh = np.zeros((33, 2, 128, 128), np.float32)
    for v in range(2):
        dist = q - k + 128 * v
        bk = _bucket(dist)
        valid = dist >= 0
        for b in range(32):
            oh[b, v] = ((bk == b) & valid).astype(np.float32)
        oh[32, v] = np.where(valid, 0.0, NEG)
    invc = np.zeros((2, 4, 128), np.float32)
    for g, w in enumerate((2, 4, 8, 16)):
        invc[0, g, :] = 1.0 / w
        invc[1, g, :] = 1.0 / np.minimum(np.arange(128) + 1, w)
    return oh.reshape(33, 2 * 128 * 128), invc.reshape(1, 2 * 4 * 128)


def build(NB, NBH, dbg=False, phases=None):
    T = NB * 128
    nc = bass.Bass("TRN2", target_bir_lowering=False)

    def din(name, shape):
        return nc.dram_tensor(name, shape, F32, kind="ExternalInput").ap()

    xin = din("xin", [T, D])
    pv_in = din("pvec", [128, 2])
    ident_in = din("ident", [128, 128])
    oh_in = din("ohtab", [33, 2 * 128 * 128])
    invc_in = din("invcnt", [1, 2 * 4 * 128])
    rel_bias = din("rel_bias", [32, 4])
    norm_mix = din("norm_mix", [2, D])
    w_in = din("w_in", [2, D, 4096])
    pool_gw = din("pool_group_w", [2, 4, 128, 128])
    pool_scale = din("pool_scale", [2, 512])
    lq1 = din("lambda_q1", [2, 64])
    lk1 = din("lambda_k1", [2, 64])
    lq2 = din("lambda_q2", [2, 64])
    lk2 = din("lambda_k2", [2, 64])
    subln = din("subln_gain", [2, 128])
    w_pu = din("w_pool_up", [2, 512, D])
    w_au = din("w_attn_up", [2, 512, D])
    w_out = din("w_out", [2, D, D])
    norm_ffn = din("norm_ffn", [2, D])
    dwg = din("dense_w_gate", [1, D, DFF])
    dwu = din("dense_w_up", [1, D, DFF])
    dwd = din("dense_w_down", [1, DFF, D])
    router = din("moe_router", [1, D, NEXP])
    mwg = din("moe_w_gate", [1, NEXP, D, DFF])
    mwu = din("moe_w_up", [1, NEXP, D, DFF])
    mwd = din("moe_w_down", [1, NEXP, DFF, D])
    final_norm = din("final_norm", [1, D])
    out = nc.dram_tensor("out", [NBH * 128, D], F32, kind="ExternalOutput").ap()

    def dscr(name, shape, dt):
        kind = "ExternalOutput" if dbg else "Internal"
        return nc.dram_tensor(name, shape, dt, kind=kind).ap()

    HNT = dscr("s_hnt", [128, 8, T], BF16)
    KT = dscr("s_kt", [4, 128, T], BF16)
    QT = dscr("s_qt", [4, 128, T], BF16)
    VV = dscr("s_vv", [T, 512], BF16)
    BO = dscr("s_bo", [T, 512], BF16)
    HM = dscr("s_hm", [2 * NBH * 128, D], F32)
    H1 = dscr("s_h1", [T, D], F32)
    BT = dscr("s_bt", [4, 2 * 128 * 128], F32)
    DBG = dscr("s_dbg", [4, NB, 128, 132], F32) if dbg else None
    DBG2 = dscr("s_dbg2", [128, 384], F32) if dbg else None

    cnt = [0]

    def uid(s):
        cnt[0] += 1
        return "%s_%d" % (s, cnt[0])

    def load_cast(P, dst, src, ncols, stg, tag, nchunk=8):
        srcv = src.rearrange("(c p) n -> p c n", p=128)
        step = max(1, 2048 // ncols)
        i = 0
        for c0 in range(0, nchunk, step):
            c1 = min(nchunk, c0 + step)
            s = stg[i % len(stg)]
            i += 1
            sv = s[:, 0:(c1 - c0) * ncols].rearrange("p (c n) -> p c n", n=ncols)
            P.dma("sp", sv, srcv[:, c0:c1, :], writes=[s.name])
            eng = "pool" if (i % 2 == 0) else "dve"
            P.add(eng, lambda e, a=dst[:, c0:c1, :], b=sv: e.tensor_copy(out=a, in_=b),
                  reads=[s.name], writes=[tag], partial=True)

    def rmsnorm_block(P, h_ap, hres, g_t, eps_t, junk, ss, rstd, hn, hnres, eps_scale=1.0 / D):
        P.add("act", lambda e: e.activation(out=junk[:], in_=h_ap, func=AF.Square, accum_out=ss[:]),
              reads=[hres], writes=[junk.name, ss.name])
        P.add("act", lambda e: e.activation(out=rstd[:], in_=ss[:], func=AF.Sqrt, bias=eps_t[:], scale=eps_scale),
              reads=[ss.name], writes=[rstd.name])
        P.add("dve", lambda e: e.reciprocal(out=rstd[:], in_=rstd[:]), reads=[rstd.name], writes=[rstd.name])
        P.add("dve", lambda e: e.scalar_tensor_tensor(out=hn[:], in0=h_ap, scalar=rstd[:, 0:1], in1=g_t[:],
                                                      op0=ALU.mult, op1=ALU.mult),
              reads=[hres, rstd.name, g_t.name], writes=[hnres])

    def phase_setup():
        P = Prog(nc)
        rb = P.sbuf("rb", [33, 4], F32)
        rb31 = P.sbuf("rb31", [33, 4], F32)
        ohs = [P.sbuf("ohs%d" % i, [33, 4096], F32) for i in range(2)]
        bts = [P.sbuf("bts%d" % i, [4, 512], F32) for i in range(2)]
        ps = [P.psum("sps%d" % i, [128, 512], F32) for i in range(2)]
        P.add("pool", lambda e: e.memset(rb[:], 1.0), writes=["rb"])
        P.dma("sp", rb[0:32, :], rel_bias, writes=["rb"])
        P.dma("sp", rb31[0:32, :], rel_bias[31:32, :].broadcast_to([32, 4]), writes=["rb31"])
        P.add("dve", lambda e: e.tensor_tensor(out=rb[0:32, :], in0=rb[0:32, :], in1=rb31[0:32, :], op=ALU.subtract),
              reads=["rb", "rb31"], writes=["rb"])
        n = 0
        for ch in range(8):
            o = ohs[ch % 2]
            P.dma("sp", o[:], oh_in[:, ch * 4096:(ch + 1) * 4096], writes=[o.name])
            for s in range(8):
                p_ = ps[n % 2]
                b_ = bts[n % 2]
                P.add("pe", lambda e, p_=p_, o=o, s=s: e.matmul(p_[0:4, :], lhsT=rb[:, :], rhs=o[:, s * 512:(s + 1) * 512],
                                                               start=True, stop=True),
                      reads=["rb", o.name], writes=[p_.name])
                P.add("dve", lambda e, p_=p_, b_=b_: e.tensor_copy(out=b_[:], in_=p_[0:4, :]), reads=[p_.name], writes=[b_.name])
                col = ch * 4096 + s * 512
                P.dma("sp", BT[:, col:col + 512], b_[:], reads=[b_.name], writes=["BT"], partial=True)
                n += 1
        P.emit()

    def phase_a(L):
        P = Prog(nc)
        src = xin if L == 0 else H1
        wqkv = P.sbuf("wqkv", [128, 8, 1536], BF16)
        stg = [P.sbuf("stgA%d" % i, [128, 2048], F32) for i in range(2)]
        g_t = P.sbuf("gA", [128, D], F32)
        eps_t = P.sbuf("epsA", [128, 1], F32)
        id32 = P.sbuf("id32A", [128, 128], F32)
        idb = P.sbuf("idbA", [128, 128], BF16)
        hts = [P.sbuf("hA%d" % i, [128, D], F32) for i in range(3)]
        junk = P.sbuf("junkA", [128, D], BF16)
        sss = [P.sbuf("ssA%d" % i, [128, 1], F32) for i in range(2)]
        rss = [P.sbuf("rsA%d" % i, [128, 1], F32) for i in range(2)]
        hns = [P.sbuf("hnA%d" % i, [128, D], BF16) for i in range(2)]
        hnTs = [P.sbuf("hnTA%d" % i, [128, 8, 128], BF16) for i in range(2)]
        kts = [P.sbuf("ktA%d" % i, [128, 4, 128], BF16) for i in range(2)]
        qts = [P.sbuf("qtA%d" % i, [128, 4, 128], BF16) for i in range(2)]
        vts = [P.sbuf("vtA%d" % i, [128, 512], BF16) for i in range(2)]
        pT = [P.psum("pTA%d" % i, [128, 512], F32) for i in range(2)]
        pK = [P.psum("pKA%d" % i, [128, 512], F32) for i in range(2)]
        pQ = [P.psum("pQA%d" % i, [128, 512], F32) for i in range(2)]
        pV = [P.psum("pVA%d" % i, [128, 512], F32) for i in range(2)]

        P.dma("sp", g_t[:], norm_mix[L:L + 1, :].broadcast_to([128, D]), writes=["gA"])
        P.add("pool", lambda e: e.memset(eps_t[:], 1e-6), writes=["epsA"])
        P.dma("sp", id32[:], ident_in, writes=["id32A"])
        P.add("dve", lambda e: e.tensor_copy(out=idb[:], in_=id32[:]), reads=["id32A"], writes=["idbA"])
        load_cast(P, wqkv, w_in[L][:, 512:2048], 1536, stg, "wqkv")
        KTv = KT.rearrange("m p t -> p m t")
        QTv = QT.rearrange("m p t -> p m t")
        for jj in range(min(2, NB)):
            P.dma("sp", hts[jj % 3][:], src[jj * 128:(jj + 1) * 128, :], writes=[hts[jj % 3].name])
        for j in range(NB):
            s = j % 2
            h, ss, rs, hn, hnT = hts[j % 3], sss[s], rss[s], hns[s], hnTs[s]
            if j + 2 < NB:
                P.dma("sp", hts[(j + 2) % 3][:], src[(j + 2) * 128:(j + 3) * 128, :], writes=[hts[(j + 2) % 3].name])
            rmsnorm_block(P, h[:], h.name, g_t, eps_t, junk, ss, rs, hn, hn.name)
            pt = pT[s]
            ptb = pt[:].bitcast(BF16)

            def tr(e, hn=hn, ptb=ptb):
                for c in range(8):
                    ins = e.transpose(out=ptb[:, c * 128:(c + 1) * 128], in_=hn[:, c * 128:(c + 1) * 128], identity=idb[:])
                return ins
            P.add("pe", tr, reads=[hn.name, "idbA"], writes=[pt.name])
            P.add("act", lambda e, hnT=hnT, ptb=ptb: e.activation(out=hnT[:].rearrange("p c t -> p (c t)"), in_=ptb, func=AF.Copy),
                  reads=[pt.name], writes=[hnT.name])
            P.dma("sp", HNT[:, :, j * 128:(j + 1) * 128], hnT[:], reads=[hnT.name], writes=["HNT"], partial=True)
            for (pp, tt, off, DR, scale, nm) in ((pQ[s], qts[s], 0, QTv, 0.125, "QT"), (pK[s], kts[s], 512, KTv, 1.0, "KT")):
                def mmf(e, pp=pp, off=off, hnT=hnT):
                    for m in range(4):
                        for k in range(8):
                            ins = e.matmul(pp[:, m * 128:(m + 1) * 128], lhsT=wqkv[:, k, off + m * 128:off + (m + 1) * 128],
                                           rhs=hnT[:, k, :], start=(k == 0), stop=(k == 7))
                    return ins
                P.add("pe", mmf, reads=["wqkv", hnT.name], writes=[pp.name])
                if nm == "QT":
                    P.add("act", lambda e, tt=tt, pp=pp, scale=scale: e.activation(out=tt[:].rearrange("p m t -> p (m t)"), in_=pp[:],
                                                                                  func=AF.Copy, scale=scale),
                          reads=[pp.name], writes=[tt.name])
                else:
                    P.add("dve", lambda e, tt=tt, pp=pp: e.tensor_copy(out=tt[:].rearrange("p m t -> p (m t)"), in_=pp[:]),
                          reads=[pp.name], writes=[tt.name])
                P.dma("sp", DR[:, :, j * 128:(j + 1) * 128], tt[:], reads=[tt.name], writes=[nm], partial=True)
            pv_, vt = pV[s], vts[s]

            def mmv(e, pv_=pv_, hnT=hnT):
                for k in range(8):
                    ins = e.matmul(pv_[:], lhsT=hnT[:, k, :], rhs=wqkv[:, k, 1024:1536], start=(k == 0), stop=(k == 7))
                return ins
            P.add("pe", mmv, reads=["wqkv", hnT.name], writes=[pv_.name])
            P.add("dve", lambda e, vt=vt, pv_=pv_: e.tensor_copy(out=vt[:], in_=pv_[:]), reads=[pv_.name], writes=[vt.name])
            P.dma("sp", VV[j * 128:(j + 1) * 128, :], vt[:], reads=[vt.name], writes=["VV"], partial=True)
        P.emit()

    def phase_b(L):
        P = Prog(nc)
        linit = LAMBDA_INIT[L]
        id32 = P.sbuf("id32B", [128, 128], F32)
        idb = P.sbuf("idbB", [128, 128], BF16)
        b32 = P.sbuf("b32B", [128, 4, 2, 128], F32)
        bia = P.sbuf("biaB", [128, 4, 2, 128], BF16)
        bfull = P.sbuf("bfullB", [128, 128], BF16)
        lam4 = P.sbuf("lam4", [128, 4, 64], F32)
        lamp = P.sbuf("lamp", [128, 2, 64], F32)
        lams = P.sbuf("lams", [128, 2], F32)
        nlam = P.sbuf("nlam", [128, 1], F32)
        sg_t = P.sbuf("sgB", [128, 128], F32)
        eps_t = P.sbuf("epsB", [128, 1], F32)
        ktb = [P.sbuf("ktB%d" % i, [128, T], BF16) for i in range(2)]
        vtb = [P.sbuf("vtB%d" % i, [128, NB, 132], BF16) for i in range(2)]
        qtb = [P.sbuf("qtB%d" % i, [128, 512], BF16) for i in range(2)]
        ptb = [P.sbuf("ptB%d" % i, [128, 2, 512], BF16) for i in range(3)]
        pS = [P.psum("pSB%d" % i, [128, 2, 512], F32) for i in range(2)]
        pO = [P.psum("pOB%d" % i, [128, 2, 256], F32) for i in range(4)]
        r12 = [P.sbuf("r12B%d" % i, [128, 2], F32) for i in range(2)]
        ot = [P.sbuf("otB%d" % i, [128, 128], F32) for i in range(2)]
        junk = P.sbuf("junkB", [128, 128], F32)
        ss2 = [P.sbuf("ss2B%d" % i, [128, 1], F32) for i in range(2)]
        rs2 = [P.sbuf("rs2B%d" % i, [128, 1], F32) for i in range(2)]
        bo_t = [P.sbuf("boB%d" % i, [128, 128], BF16) for i in range(2)]

        P.dma("sp", id32[:], ident_in, writes=["id32B"])
        P.add("dve", lambda e: e.tensor_copy(out=idb[:], in_=id32[:]), reads=["id32B"], writes=["idbB"])
        P.dma("sp", b32[:], BT.rearrange("h (v k q) -> k h v q", v=2, k=128), writes=["b32B"])
        P.add("dve", lambda e: e.tensor_copy(out=bia[:], in_=b32[:]), reads=["b32B"], writes=["biaB"])
        P.add("pool", lambda e: e.memset(bfull[:], NEG), writes=["bfullB"])
        P.add("pool", lambda e: e.memset(eps_t[:], 1e-5), writes=["epsB"])
        for i, lv in enumerate((lq1, lk1, lq2, lk2)):
            P.dma("sp", lam4[:, i, :], lv[L:L + 1, :].broadcast_to([128, 64]), writes=["lam4"], partial=True)
        P.dma("sp", sg_t[:], subln[L:L + 1, :].broadcast_to([128, 128]), writes=["sgB"])
        if dbg and L == 0:
            P.dma("sp", DBG2[:, 0:256], lam4[:].rearrange("p a b -> p (a b)"), reads=["lam4"], writes=["DBG2"], partial=True)
        P.add("dve", lambda e: e.tensor_tensor(out=lamp[:, 0, :], in0=lam4[:, 0, :], in1=lam4[:, 1, :], op=ALU.mult),
              reads=["lam4"], writes=["lamp"])
        P.add("dve", lambda e: e.tensor_tensor(out=lamp[:, 1, :], in0=lam4[:, 2, :], in1=lam4[:, 3, :], op=ALU.mult),
              reads=["lam4", "lamp"], writes=["lamp"])
        for i in range(2):
            P.add("act", lambda e, i=i: e.activation(out=lam4[:, i, :], in_=lamp[:, i, :], func=AF.Copy, accum_out=lams[:, i:i + 1]),
                  reads=["lamp"], writes=["lams", "lam4"])
        P.add("act", lambda e: e.activation(out=lams[:], in_=lams[:], func=AF.Exp), reads=["lams"], writes=["lams"])
        P.add("dve", lambda e: e.tensor_tensor(out=nlam[:], in0=lams[:, 1:2], in1=lams[:, 0:1], op=ALU.subtract),
              reads=["lams"], writes=["nlam"])
        P.add("dve", lambda e: e.tensor_scalar(out=nlam[:], in0=nlam[:], scalar1=-linit, scalar2=0.0, op0=ALU.add, op1=ALU.add),
              reads=["nlam"], writes=["nlam"])
        P.add("dve", lambda e: e.tensor_scalar(out=sg_t[:], in0=sg_t[:], scalar1=(1.0 - linit), scalar2=0.0, op0=ALU.mult, op1=ALU.add),
              reads=["sgB"], writes=["sgB"])
        for i in range(2):
            P.add("pool", lambda e, i=i: e.memset(vtb[i][:], 1.0), writes=[vtb[i].name])
        VVv = VV.rearrange("(n p) (h d) -> p n h d", p=128, h=4)
        ngroups = (NB + 3) // 4
        sidx = 0
        qidx = 0
        eidx = 0
        for h in range(4):
            kt, vt = ktb[h % 2], vtb[h % 2]
            P.dma("sp", kt[:], KT[h], writes=[kt.name])
            for n0 in range(0, NB, 8):
                n1 = min(NB, n0 + 8)
                P.dma("sp", vt[:, n0:n1, 0:128], VVv[:, n0:n1, h, :], writes=[vt.name], partial=True)
            for g in range(ngroups):
                qb0 = g * 4
                nq = min(4, NB - qb0)
                W = nq * 128
                qt = qtb[qidx % 2]
                qidx += 1
                P.dma("sp", qt[:, 0:W], QT[h][:, qb0 * 128:qb0 * 128 + W], writes=[qt.name])
                last_kb = qb0 + nq - 1
                for qi in range(nq):
                    P.add("dve", lambda e, qi=qi: e.memset(pO[qi][:], 0.0), writes=["pO%d_0" % qi, "pO%d_1" % qi])
                def emit_s(kb, slS, slP, kt=kt, qt=qt, W=W, qb0=qb0, nq=nq, h=h):
                    S2 = pS[slS]
                    pt2 = ptb[slP]

                    def smm(e, kb=kb):
                        for c in range(2):
                            extra = []
                            if kb >= qb0 - 1:
                                for qi in range(nq):
                                    rel = qb0 + qi - kb
                                    if rel >= 2:
                                        continue
                                    if rel == 1:
                                        extra.append((qi, bia[:, h, 1, :]))
                                    elif rel == 0:
                                        extra.append((qi, bia[:, h, 0, :]))
                                    else:
                                        extra.append((qi, bfull[:]))
                            ins = e.matmul(S2[:, c, 0:W], lhsT=kt[c * 64:(c + 1) * 64, kb * 128:(kb + 1) * 128],
                                           rhs=qt[c * 64:(c + 1) * 64, 0:W], start=True, stop=(len(extra) == 0))
                            for n_, (qi, bap) in enumerate(extra):
                                ins = e.matmul(S2[:, c, qi * 128:(qi + 1) * 128], lhsT=idb[:], rhs=bap, start=False,
                                               stop=(n_ == len(extra) - 1))
                        return ins
                    P.add("pe", smm, reads=[kt.name, qt.name, "biaB", "bfullB", "idbB"], writes=[S2.name])
                    P.add("act", lambda e: e.activation(out=pt2[:, :, 0:W], in_=S2[:, :, 0:W], func=AF.Exp),
                          reads=[S2.name], writes=[pt2.name])

                def emit_pv(kb, slP, vt=vt, qb0=qb0, nq=nq):
                    pt2 = ptb[slP]

                    def pvmm(e, kb=kb):
                        ins = None
                        for c in range(2):
                            for qi in range(nq):
                                qb = qb0 + qi
                                if kb > qb:
                                    continue
                                ins = e.matmul(pO[qi][:, c, 0:129], lhsT=pt2[:, c, qi * 128:(qi + 1) * 128], rhs=vt[:, kb, 0:129],
                                               start=False, stop=(kb == qb), skip_group_check=True)
                        return ins
                    P.add("pe", pvmm, reads=[pt2.name, vt.name],
                          writes=["pO%d_%d" % (qi, c) for c in range(2) for qi in range(nq) if kb <= qb0 + qi])

                prev = None
                for kb in range(last_kb + 1):
                    slS = sidx % 2
                    slP = sidx % 3
                    sidx += 1
                    emit_s(kb, slS, slP)
                    if prev is not None:
                        emit_pv(*prev)
                    prev = (kb, slP)
                emit_pv(*prev)
                for qi in range(nq):
                    qb = qb0 + qi
                    es = eidx % 2
                    eidx += 1
                    r, o, s2, rr, bo = r12[es], ot[es], ss2[es], rs2[es], bo_t[es]
                    po = pO[qi]
                    rn = ["pO%d_0" % qi, "pO%d_1" % qi]
                    P.add("dve", lambda e, r=r, po=po: e.reciprocal(out=r[:].rearrange("p (c o) -> p c o", o=1), in_=po[:, :, 128:129]),
                          reads=rn, writes=[r.name])
                    P.add("dve", lambda e, r=r: e.tensor_tensor(out=r[:, 1:2], in0=r[:, 1:2], in1=nlam[:], op=ALU.mult),
                          reads=[r.name, "nlam"], writes=[r.name])
                    P.add("dve", lambda e, r=r, po=po, o=o: e.tensor_scalar(out=o[:], in0=po[:, 0, 0:128], scalar1=r[:, 0:1], scalar2=0.0,
                                                                          op0=ALU.mult, op1=ALU.add),
                          reads=[rn[0], r.name], writes=[o.name])
                    P.add("dve", lambda e, r=r, po=po, o=o: e.scalar_tensor_tensor(out=o[:], in0=po[:, 1, 0:128], scalar=r[:, 1:2], in1=o[:],
                                                                                 op0=ALU.mult, op1=ALU.add),
                          reads=[rn[1], r.name, o.name], writes=[o.name])
                    if dbg and L == 0:
                        P.dma("sp", DBG[h, qb, :, 0:128], o[:], reads=[o.name], writes=["DBG"], partial=True)
                        P.dma("sp", DBG[h, qb, :, 128:130], r[:], reads=[r.name], writes=["DBG"], partial=True)
                        P.dma("sp", DBG[h, qb, :, 130:131], nlam[:], reads=["nlam"], writes=["DBG"], partial=True, allow_slow_non_contiguous=True)
                    P.add("dve", lambda e, o=o, s2=s2: e.scalar_tensor_tensor(out=junk[:], in0=o[:], scalar=1.0, in1=o[:], op0=ALU.mult, op1=ALU.mult,
                                                                            accum_out=s2[:]),
                          reads=[o.name], writes=["junkB", s2.name])
                    P.add("act", lambda e, s2=s2, rr=rr: e.activation(out=rr[:], in_=s2[:], func=AF.Ln, bias=eps_t[:], scale=1.0 / 128),
                          reads=[s2.name, "epsB"], writes=[rr.name])
                    P.add("act", lambda e, rr=rr: e.activation(out=rr[:], in_=rr[:], func=AF.Exp, scale=-0.5), reads=[rr.name], writes=[rr.name])
                    P.add("dve", lambda e, o=o, rr=rr, bo=bo: e.scalar_tensor_tensor(out=bo[:], in0=o[:], scalar=rr[:, 0:1], in1=sg_t[:],
                                                                                   op0=ALU.mult, op1=ALU.mult),
                          reads=[o.name, rr.name, "sgB"], writes=[bo.name])
                    P.dma("sp", BO[qb * 128:(qb + 1) * 128, h * 128:(h + 1) * 128], bo[:], reads=[bo.name], writes=["BO"], partial=True)
        P.emit()

    def phase_c(L):
        P = Prog(nc)
        src = xin if L == 0 else H1
        stg = [P.sbuf("stgC%d" % i, [128, 2048], F32) for i in range(2)]
        wu_ = P.sbuf("wuC", [128, 8, 512], BF16)
        wgt = P.sbuf("wgtC", [128, 8, 2048], BF16)
        wpu = P.sbuf("wpuC", [128, 4, D], BF16)
        wau = P.sbuf("wauC", [128, 4, D], BF16)
        wo = P.sbuf("woC", [128, 8, D], BF16)
        gw = P.sbuf("gwC", [128, 4, 128], BF16)
        psc = P.sbuf("pscC", [128, 4], F32)
        inv = P.sbuf("invC", [128, 2, 4, 128], F32)
        id32 = P.sbuf("id32C", [128, 128], F32)
        idb = P.sbuf("idbC", [128, 128], BF16)
        zt = P.sbuf("ztC", [128, D], F32)
        hx = [P.sbuf("hxC%d" % i, [128, 8, 144], BF16) for i in range(2)]
        hts = [P.sbuf("hC%d" % i, [128, D], F32) for i in range(3)]
        bos = [P.sbuf("boC%d" % i, [128, 512], BF16) for i in range(2)]
        u32s = [P.sbuf("u32C%d" % i, [128, 4, 144], F32) for i in range(2)]
        s2 = P.sbuf("s2C", [128, 4, 144], F32)
        s4 = P.sbuf("s4C", [128, 4, 144], F32)
        s8 = P.sbuf("s8C", [128, 4, 144], F32)
        s16 = P.sbuf("s16C", [128, 4, 144], F32)
        avg = P.sbuf("avgC", [128, 4, 128], F32)
        mixs = [P.sbuf("mixC%d" % i, [128, 4, 128], BF16) for i in range(2)]
        aTs = [P.sbuf("aTC%d" % i, [128, 4, 128], BF16) for i in range(2)]
        boTs = [P.sbuf("boTC%d" % i, [128, 4, 128], BF16) for i in range(2)]
        sig0s = [P.sbuf("sig0C%d" % i, [128, D], F32) for i in range(2)]
        sig1s = [P.sbuf("sig1C%d" % i, [128, D], F32) for i in range(2)]
        m0 = P.sbuf("m0C", [128, D], F32)
        m1s = [P.sbuf("m1C%d" % i, [128, D], F32) for i in range(2)]
        mgs = [P.sbuf("mgC%d" % i, [128, D], BF16) for i in range(2)]
        mgTs = [P.sbuf("mgTC%d" % i, [128, 8, 128], BF16) for i in range(2)]
        hms = [P.sbuf("hmC%d" % i, [128, D], F32) for i in range(2)]
        pU = [P.psum("pUC%d" % i, [128, 512], F32) for i in range(2)]
        pY = P.psum("pYC", [128, 512], F32)
        pG = P.psum("pGC", [128, 1024], F32)
        pX = P.psum("pXC", [128, 1024], F32)
        pTr = P.psum("pTrC", [128, 512], F32)

        P.dma("sp", id32[:], ident_in, writes=["id32C"])
        P.add("dve", lambda e: e.tensor_copy(out=idb[:], in_=id32[:]), reads=["id32C"], writes=["idbC"])
        for g in range(4):
            P.dma("sp", psc[:, g:g + 1], pool_scale[L, g * 128:(g + 1) * 128].rearrange("(p o) -> p o", o=1), writes=["pscC"], partial=True)
        P.dma("sp", inv[:].rearrange("p a g t -> p (a g t)"), invc_in.broadcast_to([128, 1024]), writes=["invC"])
        P.add("pool", lambda e: e.memset(zt[:], 0.0), writes=["ztC"])
        load_cast(P, wu_, w_in[L][:, 0:512], 512, stg, "wuC")
        load_cast(P, wgt, w_in[L][:, 2048:4096], 2048, stg, "wgtC")
        load_cast(P, wpu, w_pu[L], D, stg, "wpuC", nchunk=4)
        load_cast(P, wau, w_au[L], D, stg, "wauC", nchunk=4)
        load_cast(P, wo, w_out[L], D, stg, "woC")
        sv = stg[0][:, 0:512].rearrange("p (g d) -> p g d", d=128)
        P.dma("sp", sv, pool_gw[L].rearrange("g c d -> c g d"), writes=[stg[0].name])
        P.add("dve", lambda e: e.tensor_copy(out=gw[:], in_=sv), reads=[stg[0].name], writes=["gwC"])
        if L == 1:
            for jb in range(NB, 2 * NBH):
                P.dma("sp", HM[jb * 128:(jb + 1) * 128, :], zt[:], reads=["ztC"], writes=["HM"], partial=True)
        def issue_loads(j):
            s = j % 2
            x_, h, bo = hx[s], hts[j % 3], bos[s]
            if j == 0:
                P.add("pool", lambda e, x_=x_: e.memset(x_[:, :, 0:16], 0.0), writes=[x_.name])
                P.dma("sp", x_[:, :, 16:144], HNT[:, :, 0:128], writes=[x_.name])
            else:
                P.dma("sp", x_[:], HNT[:, :, j * 128 - 16:(j + 1) * 128], writes=[x_.name])
            P.dma("sp", h[:], src[j * 128:(j + 1) * 128, :], writes=[h.name])
            P.dma("sp", bo[:], BO[j * 128:(j + 1) * 128, :], writes=[bo.name])

        ptr_b = pTr[:].bitcast(BF16)

        def slot(j):
            s = j % 2
            return dict(x_=hx[s], h=hts[j % 3], bo=bos[s], hm=hms[s], u32=u32s[s], mix=mixs[s], aT=aTs[s], boT=boTs[s], sig0=sig0s[s],
                        sig1=sig1s[s], m1=m1s[s], mg=mgs[s], mgT=mgTs[s])

        def gmm(e, x_, off):
            for n in range(2):
                for k in range(8):
                    ins = e.matmul(pG[:, n * 512:(n + 1) * 512], lhsT=x_[:, k, 16:144], rhs=wgt[:, k, off + n * 512:off + (n + 1) * 512],
                                   start=(k == 0), stop=(k == 7))
            return ins

        def e_umm(j):
            t = slot(j)
            x_, u32 = t["x_"], t["u32"]
            for half in range(2):
                pu = pU[half]

                def umm(e, pu=pu, half=half):
                    for gg in range(2):
                        g = half * 2 + gg
                        for k in range(8):
                            ins = e.matmul(pu[:, gg * 144:(gg + 1) * 144], lhsT=wu_[:, k, g * 128:(g + 1) * 128], rhs=x_[:, k, :],
                                           start=(k == 0), stop=(k == 7))
                    return ins
                P.add("pe", umm, reads=["wuC", x_.name], writes=[pu.name])
                P.add("act", lambda e, pu=pu, half=half: e.activation(out=u32[:, half * 2:half * 2 + 2, :].rearrange("p g t -> p (g t)"),
                                                                    in_=pu[:, 0:288], func=AF.Copy),
                      reads=[pu.name], writes=[u32.name])

        def e_gate(j, which):
            t = slot(j)
            x_ = t["x_"]
            sg = t["sig0"] if which == 0 else t["sig1"]
            P.add("pe", lambda e: gmm(e, x_, which * 1024), reads=["wgtC", x_.name], writes=["pGC"])
            P.add("act", lambda e: e.activation(out=sg[:], in_=pG[:], func=AF.Sigmoid), reads=["pGC"], writes=[sg.name])

        def e_trb(j):
            t = slot(j)
            bo, boT = t["bo"], t["boT"]

            def trb(e):
                for c in range(4):
                    ins = e.transpose(out=ptr_b[:, c * 128:(c + 1) * 128], in_=bo[:, c * 128:(c + 1) * 128], identity=idb[:])
                return ins
            P.add("pe", trb, reads=[bo.name, "idbC"], writes=["pTrC"])
            P.add("dve", lambda e: e.tensor_copy(out=boT[:].rearrange("p c t -> p (c t)"), in_=ptr_b[:, 0:512]), reads=["pTrC"], writes=[boT.name])

        def e_pool(j):
            t = slot(j)
            u32, mix = t["u32"], t["mix"]
            P.add("dve", lambda e: e.tensor_tensor(out=s2[:, :, 1:144], in0=u32[:, :, 1:144], in1=u32[:, :, 0:143], op=ALU.add),
                  reads=[u32.name], writes=["s2C"])
            P.add("dve", lambda e: e.tensor_tensor(out=s4[:, 1:4, 3:144], in0=s2[:, 1:4, 3:144], in1=s2[:, 1:4, 1:142], op=ALU.add),
                  reads=["s2C"], writes=["s4C"])
            P.add("dve", lambda e: e.tensor_tensor(out=s8[:, 2:4, 7:144], in0=s4[:, 2:4, 7:144], in1=s4[:, 2:4, 3:140], op=ALU.add),
                  reads=["s4C"], writes=["s8C"])
            P.add("dve", lambda e: e.tensor_tensor(out=s16[:, 3:4, 15:144], in0=s8[:, 3:4, 15:144], in1=s8[:, 3:4, 7:136], op=ALU.add),
                  reads=["s8C"], writes=["s16C"])
            iv = 1 if j == 0 else 0
            for g, (st, sn) in enumerate(((s2, "s2C"), (s4, "s4C"), (s8, "s8C"), (s16, "s16C"))):
                P.add("dve", lambda e, g=g, st=st: e.tensor_tensor(out=avg[:, g, :], in0=st[:, g, 16:144], in1=inv[:, iv, g, :], op=ALU.mult),
                      reads=[sn, "invC"], writes=["avgC"])
            P.add("dve", lambda e: e.tensor_tensor(out=mix[:], in0=avg[:], in1=u32[:, :, 16:144], op=ALU.subtract),
                  reads=["avgC", u32.name], writes=[mix.name])

        def l_ymm(j):
            t = slot(j)
            mix, aT = t["mix"], t["aT"]

            def ymm(e):
                for g in range(4):
                    ins = e.matmul(pY[:, g * 128:(g + 1) * 128], lhsT=gw[:, g, :], rhs=mix[:, g, :], start=True, stop=True)
                return ins
            P.add("pe", ymm, reads=["gwC", mix.name], writes=["pYC"])
            for g in range(4):
                P.add("dve", lambda e, g=g: e.tensor_scalar(out=aT[:, g, :], in0=pY[:, g * 128:(g + 1) * 128], scalar1=psc[:, g:g + 1], scalar2=0.0,
                                                            op0=ALU.mult, op1=ALU.add),
                      reads=["pYC", "pscC"], writes=[aT.name])

        def l_aup(j):
            t = slot(j)
            aT, sig0 = t["aT"], t["sig0"]

            def aup(e):
                for n in range(2):
                    for g in range(4):
                        ins = e.matmul(pX[:, n * 512:(n + 1) * 512], lhsT=aT[:, g, :], rhs=wpu[:, g, n * 512:(n + 1) * 512],
                                       start=(g == 0), stop=(g == 3))
                return ins
            P.add("pe", aup, reads=["wpuC", aT.name], writes=["pXC"])
            P.add("dve", lambda e: e.tensor_tensor(out=m0[:], in0=sig0[:], in1=pX[:], op=ALU.mult), reads=[sig0.name, "pXC"], writes=["m0C"])

        def l_bup(j):
            t = slot(j)
            boT, sig1, m1, mg = t["boT"], t["sig1"], t["m1"], t["mg"]

            def bup(e):
                for n in range(2):
                    for g in range(4):
                        ins = e.matmul(pX[:, n * 512:(n + 1) * 512], lhsT=boT[:, g, :], rhs=wau[:, g, n * 512:(n + 1) * 512],
                                       start=(g == 0), stop=(g == 3))
                return ins
            P.add("pe", bup, reads=["wauC", boT.name], writes=["pXC"])
            P.add("dve", lambda e: e.tensor_tensor(out=m1[:], in0=sig1[:], in1=pX[:], op=ALU.mult), reads=[sig1.name, "pXC"], writes=[m1.name])
            P.add("dve", lambda e: e.tensor_tensor(out=mg[:], in0=m0[:], in1=m1[:], op=ALU.add), reads=["m0C", m1.name], writes=[mg.name])

        def l_trm(j):
            t = slot(j)
            mg, mgT = t["mg"], t["mgT"]

            def trm(e):
                for c in range(8):
                    ins = e.transpose(out=ptr_b[:, c * 128:(c + 1) * 128], in_=mg[:, c * 128:(c + 1) * 128], identity=idb[:])
                return ins
            P.add("pe", trm, reads=[mg.name, "idbC"], writes=["pTrC"])
            P.add("act", lambda e: e.activation(out=mgT[:].rearrange("p c t -> p (c t)"), in_=ptr_b, func=AF.Copy),
                  reads=["pTrC"], writes=[mgT.name])

        def l_omm(j):
            t = slot(j)
            mgT, h, hm = t["mgT"], t["h"], t["hm"]

            def omm(e):
                for n in range(2):
                    for k in range(8):
                        ins = e.matmul(pX[:, n * 512:(n + 1) * 512], lhsT=mgT[:, k, :], rhs=wo[:, k, n * 512:(n + 1) * 512],
                                       start=(k == 0), stop=(k == 7))
                return ins
            P.add("pe", omm, reads=["woC", mgT.name], writes=["pXC"])
            P.add("dve", lambda e: e.tensor_tensor(out=hm[:], in0=pX[:], in1=h[:], op=ALU.add), reads=["pXC", h.name], writes=[hm.name])
            P.dma("sp", HM[j * 128:(j + 1) * 128, :], hm[:], reads=[hm.name], writes=["HM"], partial=True)

        issue_loads(0)
        for j in range(NB + 1):
            je, jl = (j if j < NB else None), (j - 1 if j >= 1 else None)
            if je is not None and je + 1 < NB:
                issue_loads(je + 1)
            if je is not None:
                e_umm(je)
            if jl is not None:
                l_ymm(jl)
            if je is not None:
                e_gate(je, 0)
            if jl is not None:
                l_aup(jl)
            if je is not None:
                e_trb(je)
            if jl is not None:
                l_bup(jl)
            if je is not None:
                e_pool(je)
                e_gate(je, 1)
            if jl is not None:
                l_trm(jl)
                l_omm(jl)
        P.emit()

    def phase_d(L):
        moe = (L == 1)
        nblk = NBH if moe else NB
        GMAX = 11
        ngr = (nblk + GMAX - 1) // GMAX
        base = nblk // ngr
        rem = nblk % ngr
        gsz = [base + (1 if i < rem else 0) for i in range(ngr)]
        GM = max(gsz)
        nexp = NEXP if moe else 1
        FC = 256
        nfc = DFF // FC
        blk0 = 0
        for gi, G in enumerate(gsz):
            P = Prog(nc)
            acc = P.sbuf("accD", [128, GM, D], F32)
            hnT = P.sbuf("hnTD", [128, 8, GM * 128], BF16)
            g_t = P.sbuf("gD", [128, D], F32)
            eps_t = P.sbuf("epsD", [128, 1], F32)
            id32 = P.sbuf("id32D", [128, 128], F32)
            idb = P.sbuf("idbD", [128, 128], BF16)
            pvt = P.sbuf("pvD", [128, 2], F32)
            hb = [P.sbuf("hbD%d" % i, [128, D], F32) for i in range(2)]
            junk = P.sbuf("junkD", [128, D], BF16)
            sss = [P.sbuf("ssD%d" % i, [128, 1], F32) for i in range(2)]
            rss = [P.sbuf("rsD%d" % i, [128, 1], F32) for i in range(2)]
            hns = [P.sbuf("hnD%d" % i, [128, D], BF16) for i in range(2)]
            sgw = [P.sbuf("sgwD%d" % i, [128, 8, FC], F32) for i in range(2)]
            suw = [P.sbuf("suwD%d" % i, [128, 8, FC], F32) for i in range(2)]
            sdw = [P.sbuf("sdwD%d" % i, [128, 2, D], F32) for i in range(2)]
            wgb = [P.sbuf("wgbD%d" % i, [128, 8, FC], BF16) for i in range(2)]
            wub = [P.sbuf("wubD%d" % i, [128, 8, FC], BF16) for i in range(2)]
            wdb = [P.sbuf("wdbD%d" % i, [128, 2, D], BF16) for i in range(2)]
            sgt = [P.sbuf("sgtD%d" % i, [128, 256], F32) for i in range(4)]
            actT = [P.sbuf("actD%d" % i, [128, 2, 256], BF16) for i in range(2)]
            pGU = [P.psum("pGUD%d" % i, [128, 2, 256], F32) for i in range(4)]
            pDN = [P.psum("pDND%d" % i, [128, 1024], F32) for i in range(2)]
            pTr_t, pR_t = pGU[2], pGU[3]
            pTr = pTr_t[:].rearrange("p a b -> p (a b)")
            pR = pR_t[:].rearrange("p a b -> p (a b)")
            if moe:
                rt32 = P.sbuf("rt32D", [128, 8, NEXP], F32)
                rtb = P.sbuf("rtbD", [128, 8, NEXP], BF16)
                cw = P.sbuf("cwD", [128, GM, NEXP], F32)
                lg = P.sbuf("lgD", [128, NEXP], F32)
                lg2 = P.sbuf("lg2D", [128, NEXP], F32)
                eq = P.sbuf("eqD", [128, NEXP], F32)
                ex = P.sbuf("exD", [128, NEXP], F32)
                mm1 = P.sbuf("mm1D", [128, 1], F32)
                mm2 = P.sbuf("mm2D", [128, 1], F32)
                sm = P.sbuf("smD", [128, 1], F32)
            if L == 1:
                gf = P.sbuf("gfD", [128, D], F32)
                P.dma("sp", gf[:], final_norm.broadcast_to([128, D]), writes=["gfD"])
            P.dma("sp", g_t[:], norm_ffn[L:L + 1, :].broadcast_to([128, D]), writes=["gD"])
            P.add("pool", lambda e: e.memset(eps_t[:], 1e-6), writes=["epsD"])
            P.dma("sp", id32[:], ident_in, writes=["id32D"])
            P.add("dve", lambda e: e.tensor_copy(out=idb[:], in_=id32[:]), reads=["id32D"], writes=["idbD"])
            P.dma("sp", pvt[:], pv_in, writes=["pvD"])
            if moe:
                P.dma("sp", rt32[:], router[0].rearrange("(c p) n -> p c n", p=128), writes=["rt32D"])
                P.add("dve", lambda e: e.tensor_copy(out=rtb[:], in_=rt32[:]), reads=["rt32D"], writes=["rtbD"])
            for i in range(G):
                jb = blk0 + i
                s = i % 2
                a_ap = acc[:, i, :]
                ares = "acc%d" % i
                if moe:
                    hA, hB = hb[0], hb[1]
                    P.dma("sp", hA[:], HM[jb * 128:(jb + 1) * 128, :], writes=[hA.name])
                    P.dma("sp", hB[:], HM[(NBH + jb) * 128:(NBH + jb + 1) * 128, :], writes=[hB.name])
                    P.add("dve", lambda e, hB=hB: e.tensor_scalar(out=hB[:], in0=hB[:], scalar1=pvt[:, 0:1], scalar2=0.0, op0=ALU.mult, op1=ALU.add),
                          reads=[hB.name, "pvD"], writes=[hB.name])
                    P.add("dve", lambda e, hA=hA, hB=hB, a_ap=a_ap: e.scalar_tensor_tensor(out=a_ap, in0=hA[:], scalar=pvt[:, 1:2], in1=hB[:],
                                                                                         op0=ALU.mult, op1=ALU.add),
                          reads=[hA.name, hB.name, "pvD"], writes=[ares])
                else:
                    P.dma("sp", a_ap, HM[jb * 128:(jb + 1) * 128, :], writes=[ares])
                ss, rs, hn = sss[s], rss[s], hns[s]
                rmsnorm_block(P, a_ap, ares, g_t, eps_t, junk, ss, rs, hn, hn.name)
                ptr_b = pTr[:].bitcast(BF16)

                def tr(e, hn=hn, ptr_b=ptr_b):
                    for c in range(8):
                        ins = e.transpose(out=ptr_b[:, c * 128:(c + 1) * 128], in_=hn[:, c * 128:(c + 1) * 128], identity=idb[:])
                    return ins
                P.add("pe", tr, reads=[hn.name, "idbD"], writes=[pTr_t.name])
                P.add("act", lambda e, i=i, ptr_b=ptr_b: e.activation(out=hnT[:, :, i * 128:(i + 1) * 128],
                                                                    in_=ptr_b.rearrange("p (c t) -> p c t", t=128), func=AF.Copy),
                      reads=[pTr_t.name], writes=["hnT%d" % i])
                if moe:
                    def rmm(e, i=i):
                        for k in range(8):
                            ins = e.matmul(pR[:, 0:NEXP], lhsT=hnT[:, k, i * 128:(i + 1) * 128], rhs=rtb[:, k, :], start=(k == 0), stop=(k == 7))
                        return ins
                    P.add("pe", rmm, reads=["rtbD", "hnT%d" % i], writes=[pR_t.name])
                    P.add("dve", lambda e: e.tensor_copy(out=lg[:], in_=pR[:, 0:NEXP]), reads=[pR_t.name], writes=["lgD"])
                    P.add("dve", lambda e: e.reduce_max(out=mm1[:], in_=lg[:], axis=AX.X), reads=["lgD"], writes=["mm1D"])
                    P.add("dve", lambda e: e.tensor_scalar(out=eq[:], in0=lg[:], scalar1=mm1[:, 0:1], scalar2=0.0, op0=ALU.is_equal, op1=ALU.add),
                          reads=["lgD", "mm1D"], writes=["eqD"])
                    P.add("dve", lambda e: e.scalar_tensor_tensor(out=lg2[:], in0=eq[:], scalar=-1e30, in1=lg[:], op0=ALU.mult, op1=ALU.add),
                          reads=["eqD", "lgD"], writes=["lg2D"])
                    P.add("dve", lambda e: e.reduce_max(out=mm2[:], in_=lg2[:], axis=AX.X), reads=["lg2D"], writes=["mm2D"])
                    P.add("dve", lambda e: e.tensor_scalar(out=eq[:], in0=lg[:], scalar1=mm2[:, 0:1], scalar2=0.0, op0=ALU.is_ge, op1=ALU.add),
                          reads=["lgD", "mm2D", "eqD"], writes=["eqD"])
                    P.add("dve", lambda e: e.tensor_scalar(out=mm1[:], in0=mm1[:], scalar1=-1.0, scalar2=0.0, op0=ALU.mult, op1=ALU.add),
                          reads=["mm1D"], writes=["mm1D"])
                    P.add("act", lambda e: e.activation(out=ex[:], in_=lg[:], func=AF.Exp, bias=mm1[:], scale=1.0),
                          reads=["lgD", "mm1D"], writes=["exD"])
                    P.add("dve", lambda e: e.tensor_tensor(out=ex[:], in0=ex[:], in1=eq[:], op=ALU.mult), reads=["exD", "eqD"], writes=["exD"])
                    P.add("dve", lambda e: e.reduce_sum(out=sm[:], in_=ex[:], axis=AX.X), reads=["exD"], writes=["smD"])
                    P.add("dve", lambda e: e.reciprocal(out=sm[:], in_=sm[:]), reads=["smD"], writes=["smD"])
                    P.add("dve", lambda e, i=i: e.tensor_scalar(out=cw[:, i, :], in0=ex[:], scalar1=sm[:, 0:1], scalar2=0.0, op0=ALU.mult, op1=ALU.add),
                          reads=["exD", "smD"], writes=["cw%d" % i])
            tiles = [(t0, min(2, G - t0)) for t0 in range(0, G, 2)]
            wi = 0
            gi_ = 0
            ti_ = 0
            pend = [None]
            for ex_i in range(nexp):
                if moe:
                    Wg, Wu, Wd = mwg[0][ex_i], mwu[0][ex_i], mwd[0][ex_i]
                else:
                    Wg, Wu, Wd = dwg[0], dwu[0], dwd[0]
                Wgv = Wg.rearrange("(c p) n -> p c n", p=128)
                Wuv = Wu.rearrange("(c p) n -> p c n", p=128)
                Wdv = Wd.rearrange("(c p) n -> p c n", p=128)
                for fc in range(nfc):
                    ws = wi % 2
                    wi += 1
                    f0 = fc * FC
                    P.dma("sp", sgw[ws][:], Wgv[:, :, f0:f0 + FC], writes=[sgw[ws].name])
                    P.dma("sp", suw[ws][:], Wuv[:, :, f0:f0 + FC], writes=[suw[ws].name])
                    P.dma("sp", sdw[ws][:], Wdv[:, fc * 2:fc * 2 + 2, :], writes=[sdw[ws].name])
                    P.add("pool", lambda e, ws=ws: e.tensor_copy(out=wgb[ws][:], in_=sgw[ws][:]), reads=[sgw[ws].name], writes=[wgb[ws].name])
                    P.add("pool", lambda e, ws=ws: e.tensor_copy(out=wub[ws][:], in_=suw[ws][:]), reads=[suw[ws].name], writes=[wub[ws].name])
                    P.add("act", lambda e, ws=ws: e.activation(out=wdb[ws][:], in_=sdw[ws][:], func=AF.Copy), reads=[sdw[ws].name], writes=[wdb[ws].name])
                    for (t0, nb_) in tiles:
                        W = nb_ * 128
                        at = actT[ti_ % 2]
                        ti_ += 1
                        for fs in range(2):
                            gu = pGU[gi_ % 4]
                            sg = sgt[gi_ % 4]
                            gi_ += 1

                            def gumm(e, gu=gu, ws=ws, fs=fs, t0=t0, W=W):
                                for which, wb in ((0, wgb[ws]), (1, wub[ws])):
                                    for k in range(8):
                                        ins = e.matmul(gu[:, which, 0:W], lhsT=wb[:, k, fs * 128:(fs + 1) * 128],
                                                       rhs=hnT[:, k, t0 * 128:t0 * 128 + W], start=(k == 0), stop=(k == 7))
                                return ins
                            P.add("pe", gumm, reads=[wgb[ws].name, wub[ws].name] + ["hnT%d" % (t0 + b) for b in range(nb_)], writes=[gu.name])
                            P.add("act", lambda e, gu=gu, sg=sg, W=W: e.activation(out=sg[:, 0:W], in_=gu[:, 0, 0:W], func=AF.Silu),
                                  reads=[gu.name], writes=[sg.name])
                            P.add("dve", lambda e, gu=gu, sg=sg, at=at, fs=fs, W=W: e.tensor_tensor(out=at[:, fs, 0:W], in0=sg[:, 0:W], in1=gu[:, 1, 0:W], op=ALU.mult),
                                  reads=[gu.name, sg.name], writes=[at.name + "_%d" % fs])

                        def emit_dn(at=at, ws=ws, t0=t0, nb_=nb_, ex_i=ex_i):
                            for b in range(nb_):
                                dn = pDN[b]
                                i = t0 + b

                                def dmm(e, dn=dn, b=b):
                                    for n in range(2):
                                        for fs in range(2):
                                            ins = e.matmul(dn[:, n * 512:(n + 1) * 512], lhsT=at[:, fs, b * 128:(b + 1) * 128],
                                                           rhs=wdb[ws][:, fs, n * 512:(n + 1) * 512], start=(fs == 0), stop=(fs == 1))
                                    return ins
                                P.add("pe", dmm, reads=[at.name + "_0", at.name + "_1", wdb[ws].name], writes=[dn.name])
                                if moe:
                                    P.add("dve", lambda e, dn=dn, i=i: e.scalar_tensor_tensor(out=acc[:, i, :], in0=dn[:], scalar=cw[:, i, ex_i:ex_i + 1],
                                                                                                in1=acc[:, i, :], op0=ALU.mult, op1=ALU.add),
                                          reads=[dn.name, "cw%d" % i, "acc%d" % i], writes=["acc%d" % i])
                                else:
                                    P.add("dve", lambda e, dn=dn, i=i: e.tensor_tensor(out=acc[:, i, :], in0=dn[:], in1=acc[:, i, :], op=ALU.add),
                                          reads=[dn.name, "acc%d" % i], writes=["acc%d" % i])
                        if pend[0] is not None:
                            pend[0]()
                        pend[0] = emit_dn
            if pend[0] is not None:
                pend[0]()
                pend[0] = None
            for i in range(G):
                jb = blk0 + i
                if L == 0:
                    P.dma("sp", H1[jb * 128:(jb + 1) * 128, :], acc[:, i, :], reads=["acc%d" % i], writes=["H1"], partial=True)
                else:
                    s = i % 2
                    ss, rs, ho = sss[s], rss[s], hb[s]
                    a_ap = acc[:, i, :]
                    ares = "acc%d" % i
                    P.add("act", lambda e, a_ap=a_ap, ss=ss: e.activation(out=junk[:], in_=a_ap, func=AF.Square, accum_out=ss[:]),
                          reads=[ares], writes=["junkD", ss.name])
                    P.add("act", lambda e, ss=ss, rs=rs: e.activation(out=rs[:], in_=ss[:], func=AF.Sqrt, bias=eps_t[:], scale=1.0 / D),
                          reads=[ss.name], writes=[rs.name])
                    P.add("dve", lambda e, rs=rs: e.reciprocal(out=rs[:], in_=rs[:]), reads=[rs.name], writes=[rs.name])
                    P.add("dve", lambda e, a_ap=a_ap, rs=rs, ho=ho: e.scalar_tensor_tensor(out=ho[:], in0=a_ap, scalar=rs[:, 0:1], in1=gf[:],
                                                                                         op0=ALU.mult, op1=ALU.mult),
                          reads=[ares, rs.name, "gfD"], writes=[ho.name])
                    P.dma("sp", out[jb * 128:(jb + 1) * 128, :], ho[:], reads=[ho.name], writes=["out"], partial=True)
            P.emit()
            blk0 += G

    plist = [("S", 0)] + [(ph, L) for L in range(2) for ph in "ABCD"]
    if phases is not None:
        plist = plist[:phases]
    for ph, L in plist:
        {"S": lambda L: phase_setup(), "A": phase_a, "B": phase_b, "C": phase_c, "D": phase_d}[ph](L)
    return nc


_IN_NAMES = ["rel_bias", "norm_mix", "w_in", "pool_group_w", "pool_scale", "lambda_q1", "lambda_k1", "lambda_q2",
             "lambda_k2", "subln_gain", "w_pool_up", "w_attn_up", "w_out", "norm_ffn", "dense_w_gate", "dense_w_up",
             "dense_w_down", "moe_router", "moe_w_gate", "moe_w_up", "moe_w_down"]


def run(inputs, dbg=False, phases=None):
    x = np.asarray(inputs["x"], np.float32)
    B, S, _ = x.shape
    meta = np.asarray(inputs["meta_tokens"], np.float32)
    Ltot = 16 + S
    NB = (Ltot + 127) // 128
    NBH = (NB + 2) // 2 if NB % 2 == 1 else NB // 2 + 1
    NBH = (NB + 1) // 2
    T = NB * 128
    ncores = 2 * B
    oh, invc = _const_tables()
    ident = np.eye(128, dtype=np.float32)
    shared = {k: np.ascontiguousarray(np.asarray(inputs[k], np.float32)) for k in _IN_NAMES}
    shared["final_norm"] = np.asarray(inputs["final_norm"], np.float32).reshape(1, D)
    shared["ident"] = ident
    shared["ohtab"] = oh
    shared["invcnt"] = invc
    in_maps = []
    for c in range(ncores):
        b, p = c // 2, c % 2
        xin = np.zeros((T, D), np.float32)
        xin[0:16] = meta
        xin[16:Ltot] = x[b]
        pv = np.zeros((128, 2), np.float32)
        pv[:, 0] = p
        pv[:, 1] = 1 - p
        m = dict(shared)
        m["xin"] = xin
        m["pvec"] = pv
        in_maps.append(m)
    nc = build(NB, NBH, dbg=dbg, phases=phases)
    res = run_bass_kernel_spmd(nc, in_maps, core_ids=list(range(ncores)))
    outs = []
    for b in range(B):
        full = np.concatenate([res.results[2 * b]["out"], res.results[2 * b + 1]["out"]], axis=0)
        outs.append(full[16:Ltot])
    o = np.stack(outs, axis=0).astype(np.float32)
    if dbg:
        return o, res
    return o


def kernel(**inputs):
    return run(inputs)
```

```python
import math
import numpy as np
import concourse.bass as bass
import concourse.mybir as mybir
from concourse.bass_utils import run_bass_kernel_spmd
from contextlib import ExitStack

F32 = mybir.dt.float32
BF16 = mybir.dt.bfloat16
AF = mybir.ActivationFunctionType
ALU = mybir.AluOpType
AX = mybir.AxisListType

ENGS = ("pe", "act", "dve", "pool", "sp")
NDMA_SEMS = 12
SELF_SYNC = ("act", "dve", "pool")


class Prog:
    def __init__(self, nc):
        self.nc = nc
        self.ops = []
        self.last_w = {}
        self.readers = {}
        self.es = ExitStack()

    _n = [0]
    G = None

    def sbuf(self, name, shape, dt):
        Prog._n[0] += 1
        return self.es.enter_context(self.nc.sbuf_tensor("%s_u%d" % (name, Prog._n[0]), shape, dt))

    def psum(self, name, shape, dt):
        Prog._n[0] += 1
        return self.es.enter_context(self.nc.psum_tensor("%s_u%d" % (name, Prog._n[0]), shape, dt))

    def add(self, eng, fn, reads=(), writes=(), dma=False, partial=False):
        idx = len(self.ops)
        deps = set()
        for r in reads:
            for w in self.last_w.get(r, ()):
                deps.add(w)
        for w in writes:
            for pw in self.last_w.get(w, ()):
                if not (partial and self.ops[pw]["partial"]):
                    deps.add(pw)
            for rd in self.readers.get(w, ()):
                deps.add(rd)
        deps.discard(idx)
        self.ops.append(dict(eng=eng, fn=fn, deps=deps, dma=dma, idx=idx, partial=partial))
        for r in reads:
            self.readers.setdefault(r, []).append(idx)
        for w in writes:
            if partial and not self.readers.get(w):
                self.last_w.setdefault(w, []).append(idx)
            else:
                self.last_w[w] = [idx]
            self.readers[w] = []
        return idx

    def dma(self, q, out, in_, reads=(), writes=(), partial=False, **kw):
        return self.add(q, lambda e: e.dma_start(out=out, in_=in_, **kw), reads, writes, dma=True, partial=partial)

    def emit(self):
        nc = self.nc
        ops = self.ops
        need = [False] * len(ops)
        for o in ops:
            for d in o["deps"]:
                po = ops[d]
                if po["dma"] or po["eng"] != o["eng"] or po["eng"] in SELF_SYNC:
                    need[d] = True
        dmaq = set(o["eng"] for o in ops if o["dma"])
        if Prog.G is None or Prog.G["nc"] is not nc:
            ges = ExitStack()
            Prog.G = dict(
                nc=nc, es=ges,
                esem={e: ges.enter_context(nc.semaphore("gs_" + e)) for e in ENGS},
                dsem={e: [ges.enter_context(nc.semaphore("gd_%s%d" % (e, i))) for i in range(NDMA_SEMS)] for e in ("sp",)},
                ecount={e: 0 for e in ENGS},
                dcount={e: [0] * NDMA_SEMS for e in ENGS},
                dn={e: 0 for e in ENGS})
        G = Prog.G
        esem, dsem, ecount, dcount, dn = G["esem"], G["dsem"], G["ecount"], G["dcount"], G["dn"]
        for o in ops:
            i = o["idx"]
            e = o["eng"]
            o["sig"] = None
            o["prewait"] = None
            if o["dma"]:
                k = dn[e] % NDMA_SEMS
                dn[e] += 1
                if dcount[e][k] > 0:
                    o["prewait"] = (dsem[e][k], dcount[e][k], ("d", e, k))
                dcount[e][k] += 16
                o["sig"] = (dsem[e][k], dcount[e][k], ("d", e, k), 16)
            elif need[i]:
                ecount[e] += 1
                o["sig"] = (esem[e], ecount[e], ("e", e), 1)
        streams = {e: [o for o in ops if o["eng"] == e] for e in ENGS}
        final_waits = {e: [] for e in ENGS}
        for e in dmaq:
            for k in range(NDMA_SEMS):
                if dcount[e][k] > 0:
                    final_waits[e].append((dsem[e][k], dcount[e][k]))

        def run_stream(e, eng):
            waited = {}
            for o in streams[e]:
                ws = []
                if o["prewait"] is not None:
                    ws.append(o["prewait"])
                for d in sorted(o["deps"]):
                    po = ops[d]
                    if po["dma"] or po["eng"] != e or e in SELF_SYNC:
                        s = po["sig"]
                        ws.append((s[0], s[1], s[2]))
                best = {}
                for (sem, val, key) in ws:
                    if key not in best or best[key][1] < val:
                        best[key] = (sem, val)
                for key, (sem, val) in best.items():
                    if waited.get(key, 0) >= val:
                        continue
                    eng.wait_ge(sem, val)
                    waited[key] = val
                ins = o["fn"](eng)
                if o["sig"] is not None:
                    ins.then_inc(o["sig"][0], o["sig"][3])
            for (sem, val) in final_waits[e]:
                eng.wait_ge(sem, val)

        with nc.Block() as block:
            @block.tensor
            def _(eng):
                run_stream("pe", eng)

            @block.scalar
            def _(eng):
                run_stream("act", eng)

            @block.vector
            def _(eng):
                run_stream("dve", eng)

            @block.gpsimd
            def _(eng):
                run_stream("pool", eng)

            @block.sync
            def _(eng):
                run_stream("sp", eng)
        self.es.close()


D = 1024
DFF = 3584
NEXP = 8
LAMBDA_INIT = [0.8 - 0.6 * math.exp(-0.3 * l) for l in range(2)]
NEG = -30000.0


def _bucket(n):
    n = np.maximum(n, 0)
    nf = np.maximum(n, 1).astype(np.float32)
    large = 16 + (np.log(nf / np.float32(16)) / np.float32(math.log(8.0)) * np.float32(16)).astype(np.int32)
    large = np.minimum(large, 31)
    return np.where(n < 16, n, large)


def _const_tables():
    k = np.arange(128)[:, None]
    q = np.arange(128)[None, :]
    oh = np.zeros((33, 2, 128, 128), np.float32)
    for v in range(2):
        dist = q - k + 128 * v
        bk = _bucket(dist)
        valid = dist >= 0
        for b in range(32):
            oh[b, v] = ((bk == b) & valid).astype(np.float32)
        oh[32, v] = np.where(valid, 0.0, NEG)
    invc = np.zeros((2, 4, 128), np.float32)
    for g, w in enumerate((2, 4, 8, 16)):
        invc[0, g, :] = 1.0 / w
        invc[1, g, :] = 1.0 / np.minimum(np.arange(128) + 1, w)
    return oh.reshape(33, 2 * 128 * 128), invc.reshape(1, 2 * 4 * 128)


def build(NB, NBH, dbg=False, phases=None):
    T = NB * 128
    nc = bass.Bass("TRN2", target_bir_lowering=False)

    def din(name, shape):
        return nc.dram_tensor(name, shape, F32, kind="ExternalInput").ap()

    xin = din("xin", [T, D])
    pv_in = din("pvec", [128, 2])
    ident_in = din("ident", [128, 128])
    oh_in = din("ohtab", [33, 2 * 128 * 128])
    invc_in = din("invcnt", [1, 2 * 4 * 128])
    rel_bias = din("rel_bias", [32, 4])
    norm_mix = din("norm_mix", [2, D])
    w_in = din("w_in", [2, D, 4096])
    pool_gw = din("pool_group_w", [2, 4, 128, 128])
    pool_scale = din("pool_scale", [2, 512])
    lq1 = din("lambda_q1", [2, 64])
    lk1 = din("lambda_k1", [2, 64])
    lq2 = din("lambda_q2", [2, 64])
    lk2 = din("lambda_k2", [2, 64])
    subln = din("subln_gain", [2, 128])
    w_pu = din("w_pool_up", [2, 512, D])
    w_au = din("w_attn_up", [2, 512, D])
    w_out = din("w_out", [2, D, D])
    norm_ffn = din("norm_ffn", [2, D])
    dwg = din("dense_w_gate", [1, D, DFF])
    dwu = din("dense_w_up", [1, D, DFF])
    dwd = din("dense_w_down", [1, DFF, D])
    router = din("moe_router", [1, D, NEXP])
    mwg = din("moe_w_gate", [1, NEXP, D, DFF])
    mwu = din("moe_w_up", [1, NEXP, D, DFF])
    mwd = din("moe_w_down", [1, NEXP, DFF, D])
    final_norm = din("final_norm", [1, D])
    out = nc.dram_tensor("out", [NBH * 128, D], F32, kind="ExternalOutput").ap()

    def dscr(name, shape, dt):
        kind = "ExternalOutput" if dbg else "Internal"
        return nc.dram_tensor(name, shape, dt, kind=kind).ap()

    HNT = dscr("s_hnt", [128, 8, T], BF16)
    KT = dscr("s_kt", [4, 128, T], BF16)
    QT = dscr("s_qt", [4, 128, T], BF16)
    VV = dscr("s_vv", [T, 512], BF16)
    BO = dscr("s_bo", [T, 512], BF16)
    HM = dscr("s_hm", [2 * NBH * 128, D], F32)
    H1 = dscr("s_h1", [T, D], F32)
    BT = dscr("s_bt", [4, 2 * 128 * 128], F32)
    DBG = dscr("s_dbg", [4, NB, 128, 132], F32) if dbg else None
    DBG2 = dscr("s_dbg2", [128, 384], F32) if dbg else None

    cnt = [0]

    def uid(s):
        cnt[0] += 1
        return "%s_%d" % (s, cnt[0])

    def load_cast(P, dst, src, ncols, stg, tag, nchunk=8):
        srcv = src.rearrange("(c p) n -> p c n", p=128)
        step = max(1, 2048 // ncols)
        i = 0
        for c0 in range(0, nchunk, step):
            c1 = min(nchunk, c0 + step)
            s = stg[i % len(stg)]
            i += 1
            sv = s[:, 0:(c1 - c0) * ncols].rearrange("p (c n) -> p c n", n=ncols)
            P.dma("sp", sv, srcv[:, c0:c1, :], writes=[s.name])
            eng = "pool" if (i % 2 == 0) else "dve"
            P.add(eng, lambda e, a=dst[:, c0:c1, :], b=sv: e.tensor_copy(out=a, in_=b),
                  reads=[s.name], writes=[tag], partial=True)

    def rmsnorm_block(P, h_ap, hres, g_t, eps_t, junk, ss, rstd, hn, hnres, eps_scale=1.0 / D):
        P.add("act", lambda e: e.activation(out=junk[:], in_=h_ap, func=AF.Square, accum_out=ss[:]),
              reads=[hres], writes=[junk.name, ss.name])
        P.add("act", lambda e: e.activation(out=rstd[:], in_=ss[:], func=AF.Sqrt, bias=eps_t[:], scale=eps_scale),
              reads=[ss.name], writes=[rstd.name])
        P.add("dve", lambda e: e.reciprocal(out=rstd[:], in_=rstd[:]), reads=[rstd.name], writes=[rstd.name])
        P.add("dve", lambda e: e.scalar_tensor_tensor(out=hn[:], in0=h_ap, scalar=rstd[:, 0:1], in1=g_t[:],
                                                      op0=ALU.mult, op1=ALU.mult),
              reads=[hres, rstd.name, g_t.name], writes=[hnres])

    def phase_setup():
        P = Prog(nc)
        rb = P.sbuf("rb", [33, 4], F32)
        rb31 = P.sbuf("rb31", [33, 4], F32)
        ohs = [P.sbuf("ohs%d" % i, [33, 4096], F32) for i in range(2)]
        bts = [P.sbuf("bts%d" % i, [4, 512], F32) for i in range(2)]
        ps = [P.psum("sps%d" % i, [128, 512], F32) for i in range(2)]
        P.add("pool", lambda e: e.memset(rb[:], 1.0), writes=["rb"])
        P.dma("sp", rb[0:32, :], rel_bias, writes=["rb"])
        P.dma("sp", rb31[0:32, :], rel_bias[31:32, :].broadcast_to([32, 4]), writes=["rb31"])
        P.add("dve", lambda e: e.tensor_tensor(out=rb[0:32, :], in0=rb[0:32, :], in1=rb31[0:32, :], op=ALU.subtract),
              reads=["rb", "rb31"], writes=["rb"])
        n = 0
        for ch in range(8):
            o = ohs[ch % 2]
            P.dma("sp", o[:], oh_in[:, ch * 4096:(ch + 1) * 4096], writes=[o.name])
            for s in range(8):
                p_ = ps[n % 2]
                b_ = bts[n % 2]
                P.add("pe", lambda e, p_=p_, o=o, s=s: e.matmul(p_[0:4, :], lhsT=rb[:, :], rhs=o[:, s * 512:(s + 1) * 512],
                                                               start=True, stop=True),
                      reads=["rb", o.name], writes=[p_.name])
                P.add("dve", lambda e, p_=p_, b_=b_: e.tensor_copy(out=b_[:], in_=p_[0:4, :]), reads=[p_.name], writes=[b_.name])
                col = ch * 4096 + s * 512
                P.dma("sp", BT[:, col:col + 512], b_[:], reads=[b_.name], writes=["BT"], partial=True)
                n += 1
        P.emit()

    def phase_a(L):
        P = Prog(nc)
        src = xin if L == 0 else H1
        wqkv = P.sbuf("wqkv", [128, 8, 1536], BF16)
        stg = [P.sbuf("stgA%d" % i, [128, 2048], F32) for i in range(2)]
        g_t = P.sbuf("gA", [128, D], F32)
        eps_t = P.sbuf("epsA", [128, 1], F32)
        id32 = P.sbuf("id32A", [128, 128], F32)
        idb = P.sbuf("idbA", [128, 128], BF16)
        hts = [P.sbuf("hA%d" % i, [128, D], F32) for i in range(3)]
        junk = P.sbuf("junkA", [128, D], BF16)
        sss = [P.sbuf("ssA%d" % i, [128, 1], F32) for i in range(2)]
        rss = [P.sbuf("rsA%d" % i, [128, 1], F32) for i in range(2)]
        hns = [P.sbuf("hnA%d" % i, [128, D], BF16) for i in range(2)]
        hnTs = [P.sbuf("hnTA%d" % i, [128, 8, 128], BF16) for i in range(2)]
        kts = [P.sbuf("ktA%d" % i, [128, 4, 128], BF16) for i in range(2)]
        qts = [P.sbuf("qtA%d" % i, [128, 4, 128], BF16) for i in range(2)]
        vts = [P.sbuf("vtA%d" % i, [128, 512], BF16) for i in range(2)]
        pT = [P.psum("pTA%d" % i, [128, 512], F32) for i in range(2)]
        pK = [P.psum("pKA%d" % i, [128, 512], F32) for i in range(2)]
        pQ = [P.psum("pQA%d" % i, [128, 512], F32) for i in range(2)]
        pV = [P.psum("pVA%d" % i, [128, 512], F32) for i in range(2)]

        P.dma("sp", g_t[:], norm_mix[L:L + 1, :].broadcast_to([128, D]), writes=["gA"])
        P.add("pool", lambda e: e.memset(eps_t[:], 1e-6), writes=["epsA"])
        P.dma("sp", id32[:], ident_in, writes=["id32A"])
        P.add("dve", lambda e: e.tensor_copy(out=idb[:], in_=id32[:]), reads=["id32A"], writes=["idbA"])
        load_cast(P, wqkv, w_in[L][:, 512:2048], 1536, stg, "wqkv")
        KTv = KT.rearrange("m p t -> p m t")
        QTv = QT.rearrange("m p t -> p m t")
        for jj in range(min(2, NB)):
            P.dma("sp", hts[jj % 3][:], src[jj * 128:(jj + 1) * 128, :], writes=[hts[jj % 3].name])
        for j in range(NB):
            s = j % 2
            h, ss, rs, hn, hnT = hts[j % 3], sss[s], rss[s], hns[s], hnTs[s]
            if j + 2 < NB:
                P.dma("sp", hts[(j + 2) % 3][:], src[(j + 2) * 128:(j + 3) * 128, :], writes=[hts[(j + 2) % 3].name])
            rmsnorm_block(P, h[:], h.name, g_t, eps_t, junk, ss, rs, hn, hn.name)
            pt = pT[s]
            ptb = pt[:].bitcast(BF16)

            def tr(e, hn=hn, ptb=ptb):
                for c in range(8):
                    ins = e.transpose(out=ptb[:, c * 128:(c + 1) * 128], in_=hn[:, c * 128:(c + 1) * 128], identity=idb[:])
                return ins
            P.add("pe", tr, reads=[hn.name, "idbA"], writes=[pt.name])
            P.add("act", lambda e, hnT=hnT, ptb=ptb: e.activation(out=hnT[:].rearrange("p c t -> p (c t)"), in_=ptb, func=AF.Copy),
                  reads=[pt.name], writes=[hnT.name])
            P.dma("sp", HNT[:, :, j * 128:(j + 1) * 128], hnT[:], reads=[hnT.name], writes=["HNT"], partial=True)
            for (pp, tt, off, DR, scale, nm) in ((pQ[s], qts[s], 0, QTv, 0.125, "QT"), (pK[s], kts[s], 512, KTv, 1.0, "KT")):
                def mmf(e, pp=pp, off=off, hnT=hnT):
                    for m in range(4):
                        for k in range(8):
                            ins = e.matmul(pp[:, m * 128:(m + 1) * 128], lhsT=wqkv[:, k, off + m * 128:off + (m + 1) * 128],
                                           rhs=hnT[:, k, :], start=(k == 0), stop=(k == 7))
                    return ins
                P.add("pe", mmf, reads=["wqkv", hnT.name], writes=[pp.name])
                if nm == "QT":
                    P.add("act", lambda e, tt=tt, pp=pp, scale=scale: e.activation(out=tt[:].rearrange("p m t -> p (m t)"), in_=pp[:],
                                                                                  func=AF.Copy, scale=scale),
                          reads=[pp.name], writes=[tt.name])
                else:
                    P.add("dve", lambda e, tt=tt, pp=pp: e.tensor_copy(out=tt[:].rearrange("p m t -> p (m t)"), in_=pp[:]),
                          reads=[pp.name], writes=[tt.name])
                P.dma("sp", DR[:, :, j * 128:(j + 1) * 128], tt[:], reads=[tt.name], writes=[nm], partial=True)
            pv_, vt = pV[s], vts[s]

            def mmv(e, pv_=pv_, hnT=hnT):
                for k in range(8):
                    ins = e.matmul(pv_[:], lhsT=hnT[:, k, :], rhs=wqkv[:, k, 1024:1536], start=(k == 0), stop=(k == 7))
                return ins
            P.add("pe", mmv, reads=["wqkv", hnT.name], writes=[pv_.name])
            P.add("dve", lambda e, vt=vt, pv_=pv_: e.tensor_copy(out=vt[:], in_=pv_[:]), reads=[pv_.name], writes=[vt.name])
            P.dma("sp", VV[j * 128:(j + 1) * 128, :], vt[:], reads=[vt.name], writes=["VV"], partial=True)
        P.emit()

    def phase_b(L):
        P = Prog(nc)
        linit = LAMBDA_INIT[L]
        id32 = P.sbuf("id32B", [128, 128], F32)
        idb = P.sbuf("idbB", [128, 128], BF16)
        b32 = P.sbuf("b32B", [128, 4, 2, 128], F32)
        bia = P.sbuf("biaB", [128, 4, 2, 128], BF16)
        bfull = P.sbuf("bfullB", [128, 128], BF16)
        lam4 = P.sbuf("lam4", [128, 4, 64], F32)
        lamp = P.sbuf("lamp", [128, 2, 64], F32)
        lams = P.sbuf("lams", [128, 2], F32)
        nlam = P.sbuf("nlam", [128, 1], F32)
        sg_t = P.sbuf("sgB", [128, 128], F32)
        eps_t = P.sbuf("epsB", [128, 1], F32)
        ktb = [P.sbuf("ktB%d" % i, [128, T], BF16) for i in range(2)]
        vtb = [P.sbuf("vtB%d" % i, [128, NB, 132], BF16) for i in range(2)]
        qtb = [P.sbuf("qtB%d" % i, [128, 512], BF16) for i in range(2)]
        ptb = [P.sbuf("ptB%d" % i, [128, 2, 512], BF16) for i in range(3)]
        pS = [P.psum("pSB%d" % i, [128, 2, 512], F32) for i in range(2)]
        pO = [P.psum("pOB%d" % i, [128, 2, 256], F32) for i in range(4)]
        osb = [P.sbuf("osbB%d" % i, [128, 2, 132], F32) for i in range(8)]
        r12 = [P.sbuf("r12B%d" % i, [128, 2], F32) for i in range(2)]
        ot = [P.sbuf("otB%d" % i, [128, 128], F32) for i in range(2)]
        junk = P.sbuf("junkB", [128, 128], F32)
        ss2 = [P.sbuf("ss2B%d" % i, [128, 1], F32) for i in range(2)]
        rs2 = [P.sbuf("rs2B%d" % i, [128, 1], F32) for i in range(2)]
        bo_t = [P.sbuf("boB%d" % i, [128, 128], BF16) for i in range(2)]

        P.dma("sp", id32[:], ident_in, writes=["id32B"])
        P.add("dve", lambda e: e.tensor_copy(out=idb[:], in_=id32[:]), reads=["id32B"], writes=["idbB"])
        P.dma("sp", b32[:], BT.rearrange("h (v k q) -> k h v q", v=2, k=128), writes=["b32B"])
        P.add("dve", lambda e: e.tensor_copy(out=bia[:], in_=b32[:]), reads=["b32B"], writes=["biaB"])
        P.add("pool", lambda e: e.memset(bfull[:], NEG), writes=["bfullB"])
        P.add("pool", lambda e: e.memset(eps_t[:], 1e-5), writes=["epsB"])
        for i, lv in enumerate((lq1, lk1, lq2, lk2)):
            P.dma("sp", lam4[:, i, :], lv[L:L + 1, :].broadcast_to([128, 64]), writes=["lam4"], partial=True)
        P.dma("sp", sg_t[:], subln[L:L + 1, :].broadcast_to([128, 128]), writes=["sgB"])
        if dbg and L == 0:
            P.dma("sp", DBG2[:, 0:256], lam4[:].rearrange("p a b -> p (a b)"), reads=["lam4"], writes=["DBG2"], partial=True)
        P.add("dve", lambda e: e.tensor_tensor(out=lamp[:, 0, :], in0=lam4[:, 0, :], in1=lam4[:, 1, :], op=ALU.mult),
              reads=["lam4"], writes=["lamp"])
        P.add("dve", lambda e: e.tensor_tensor(out=lamp[:, 1, :], in0=lam4[:, 2, :], in1=lam4[:, 3, :], op=ALU.mult),
              reads=["lam4", "lamp"], writes=["lamp"])
        for i in range(2):
            P.add("act", lambda e, i=i: e.activation(out=lam4[:, i, :], in_=lamp[:, i, :], func=AF.Copy, accum_out=lams[:, i:i + 1]),
                  reads=["lamp"], writes=["lams", "lam4"])
        P.add("act", lambda e: e.activation(out=lams[:], in_=lams[:], func=AF.Exp), reads=["lams"], writes=["lams"])
        P.add("dve", lambda e: e.tensor_tensor(out=nlam[:], in0=lams[:, 1:2], in1=lams[:, 0:1], op=ALU.subtract),
              reads=["lams"], writes=["nlam"])
        P.add("dve", lambda e: e.tensor_scalar(out=nlam[:], in0=nlam[:], scalar1=-linit, scalar2=0.0, op0=ALU.add, op1=ALU.add),
              reads=["nlam"], writes=["nlam"])
        P.add("dve", lambda e: e.tensor_scalar(out=sg_t[:], in0=sg_t[:], scalar1=(1.0 - linit), scalar2=0.0, op0=ALU.mult, op1=ALU.add),
              reads=["sgB"], writes=["sgB"])
        for i in range(2):
            P.add("pool", lambda e, i=i: e.memset(vtb[i][:], 1.0), writes=[vtb[i].name])
        VVv = VV.rearrange("(n p) (h d) -> p n h d", p=128, h=4)
        ngroups = (NB + 3) // 4
        sidx = 0
        qidx = 0
        eidx = 0
        def head_loads(h):
            kt, vt = ktb[h % 2], vtb[h % 2]
            P.dma("sp", kt[:], KT[h], writes=[kt.name])
            for n0 in range(0, NB, 8):
                n1 = min(NB, n0 + 8)
                P.dma("sp", vt[:, n0:n1, 0:128], VVv[:, n0:n1, h, :], writes=[vt.name], partial=True)

        for qi in range(4):
            P.add("dve", lambda e, qi=qi: e.memset(pO[qi][:], 0.0), writes=["pO%d_0" % qi, "pO%d_1" % qi])
        head_loads(0)
        pend_epi = [None]
        oidx = 0
        eidx_box = [0]
        for h in range(4):
            kt, vt = ktb[h % 2], vtb[h % 2]
            if h + 1 < 4:
                head_loads(h + 1)
            for g in range(ngroups):
                qb0 = g * 4
                nq = min(4, NB - qb0)
                W = nq * 128
                qt = qtb[qidx % 2]
                qidx += 1
                P.dma("sp", qt[:, 0:W], QT[h][:, qb0 * 128:qb0 * 128 + W], writes=[qt.name])
                last_kb = qb0 + nq - 1
                def emit_s(kb, slS, slP, kt=kt, qt=qt, W=W, qb0=qb0, nq=nq, h=h):
                    S2 = pS[slS]
                    pt2 = ptb[slP]

                    def smm(e, kb=kb):
                        for c in range(2):
                            extra = []
                            if kb >= qb0 - 1:
                                for qi in range(nq):
                                    rel = qb0 + qi - kb
                                    if rel >= 2:
                                        continue
                                    if rel == 1:
                                        extra.append((qi, bia[:, h, 1, :]))
                                    elif rel == 0:
                                        extra.append((qi, bia[:, h, 0, :]))
                                    else:
                                        extra.append((qi, bfull[:]))
                            ins = e.matmul(S2[:, c, 0:W], lhsT=kt[c * 64:(c + 1) * 64, kb * 128:(kb + 1) * 128],
                                           rhs=qt[c * 64:(c + 1) * 64, 0:W], start=True, stop=(len(extra) == 0))
                            for n_, (qi, bap) in enumerate(extra):
                                ins = e.matmul(S2[:, c, qi * 128:(qi + 1) * 128], lhsT=idb[:], rhs=bap, start=False,
                                               stop=(n_ == len(extra) - 1))
                        return ins
                    P.add("pe", smm, reads=[kt.name, qt.name, "biaB", "bfullB", "idbB"], writes=[S2.name])
                    P.add("act", lambda e: e.activation(out=pt2[:, :, 0:W], in_=S2[:, :, 0:W], func=AF.Exp),
                          reads=[S2.name], writes=[pt2.name])

                def emit_pv(kb, slP, vt=vt, qb0=qb0, nq=nq):
                    pt2 = ptb[slP]

                    def pvmm(e, kb=kb):
                        ins = None
                        for c in range(2):
                            for qi in range(nq):
                                qb = qb0 + qi
                                if kb > qb:
                                    continue
                                ins = e.matmul(pO[qi][:, c, 0:129], lhsT=pt2[:, c, qi * 128:(qi + 1) * 128], rhs=vt[:, kb, 0:129],
                                               start=False, stop=(kb == qb), skip_group_check=True)
                        return ins
                    P.add("pe", pvmm, reads=[pt2.name, vt.name],
                          writes=["pO%d_%d" % (qi, c) for c in range(2) for qi in range(nq) if kb <= qb0 + qi])

                pendq = []
                for kb in range(last_kb + 1):
                    slS = sidx % 2
                    slP = sidx % 3
                    sidx += 1
                    emit_s(kb, slS, slP)
                    pendq.append((kb, slP))
                    if len(pendq) > 2:
                        emit_pv(*pendq.pop(0))
                    if kb == 3 and pend_epi[0] is not None:
                        pend_epi[0]()
                        pend_epi[0] = None
                while pendq:
                    emit_pv(*pendq.pop(0))
                if pend_epi[0] is not None:
                    pend_epi[0]()
                    pend_epi[0] = None
                oslots = []
                for qi in range(nq):
                    ob = osb[oidx % 8]
                    oidx += 1
                    oslots.append(ob)
                    rn = ["pO%d_0" % qi, "pO%d_1" % qi]
                    P.add("dve", lambda e, ob=ob, qi=qi: e.tensor_copy(out=ob[:, :, 0:129], in_=pO[qi][:, :, 0:129]), reads=rn, writes=[ob.name])
                    P.add("dve", lambda e, qi=qi: e.memset(pO[qi][:], 0.0), writes=rn)

                def epi(oslots=oslots, qb0=qb0, nq=nq, h=h):
                    nonlocal_e = eidx_box
                    for qi in range(nq):
                        qb = qb0 + qi
                        es = nonlocal_e[0] % 2
                        nonlocal_e[0] += 1
                        r, o, s2, rr, bo = r12[es], ot[es], ss2[es], rs2[es], bo_t[es]
                        ob = oslots[qi]
                        P.add("dve", lambda e, r=r, ob=ob: e.reciprocal(out=r[:].rearrange("p (c o) -> p c o", o=1), in_=ob[:, :, 128:129]),
                              reads=[ob.name], writes=[r.name])
                        P.add("dve", lambda e, r=r: e.tensor_tensor(out=r[:, 1:2], in0=r[:, 1:2], in1=nlam[:], op=ALU.mult),
                              reads=[r.name, "nlam"], writes=[r.name])
                        P.add("dve", lambda e, r=r, ob=ob, o=o: e.tensor_scalar(out=o[:], in0=ob[:, 0, 0:128], scalar1=r[:, 0:1], scalar2=0.0,
                                                                              op0=ALU.mult, op1=ALU.add),
                              reads=[ob.name, r.name], writes=[o.name])
                        P.add("dve", lambda e, r=r, ob=ob, o=o: e.scalar_tensor_tensor(out=o[:], in0=ob[:, 1, 0:128], scalar=r[:, 1:2], in1=o[:],
                                                                                     op0=ALU.mult, op1=ALU.add),
                              reads=[ob.name, r.name, o.name], writes=[o.name])
                        P.add("dve", lambda e, o=o, s2=s2: e.scalar_tensor_tensor(out=junk[:], in0=o[:], scalar=1.0, in1=o[:], op0=ALU.mult, op1=ALU.mult,
                                                                                accum_out=s2[:]),
                              reads=[o.name], writes=["junkB", s2.name])
                        P.add("act", lambda e, s2=s2, rr=rr: e.activation(out=rr[:], in_=s2[:], func=AF.Ln, bias=eps_t[:], scale=1.0 / 128),
                              reads=[s2.name, "epsB"], writes=[rr.name])
                        P.add("act", lambda e, rr=rr: e.activation(out=rr[:], in_=rr[:], func=AF.Exp, scale=-0.5), reads=[rr.name], writes=[rr.name])
                        P.add("dve", lambda e, o=o, rr=rr, bo=bo: e.scalar_tensor_tensor(out=bo[:], in0=o[:], scalar=rr[:, 0:1], in1=sg_t[:],
                                                                                       op0=ALU.mult, op1=ALU.mult),
                              reads=[o.name, rr.name, "sgB"], writes=[bo.name])
                        P.dma("sp", BO[qb * 128:(qb + 1) * 128, h * 128:(h + 1) * 128], bo[:], reads=[bo.name], writes=["BO"], partial=True)
                pend_epi[0] = epi
        if pend_epi[0] is not None:
            pend_epi[0]()
            pend_epi[0] = None
        P.emit()

    def phase_c(L):
        P = Prog(nc)
        src = xin if L == 0 else H1
        stg = [P.sbuf("stgC%d" % i, [128, 2048], F32) for i in range(2)]
        wu_ = P.sbuf("wuC", [128, 8, 512], BF16)
        wgt = P.sbuf("wgtC", [128, 8, 2048], BF16)
        wpu = P.sbuf("wpuC", [128, 4, D], BF16)
        wau = P.sbuf("wauC", [128, 4, D], BF16)
        wo = P.sbuf("woC", [128, 8, D], BF16)
        gw = P.sbuf("gwC", [128, 4, 128], BF16)
        psc = P.sbuf("pscC", [128, 4], F32)
        inv = P.sbuf("invC", [128, 2, 4, 128], F32)
        id32 = P.sbuf("id32C", [128, 128], F32)
        idb = P.sbuf("idbC", [128, 128], BF16)
        zt = P.sbuf("ztC", [128, D], F32)
        hx = [P.sbuf("hxC%d" % i, [128, 8, 144], BF16) for i in range(2)]
        hts = [P.sbuf("hC%d" % i, [128, D], F32) for i in range(3)]
        bos = [P.sbuf("boC%d" % i, [128, 512], BF16) for i in range(2)]
        u32s = [P.sbuf("u32C%d" % i, [128, 4, 144], F32) for i in range(2)]
        s2 = P.sbuf("s2C", [128, 4, 144], F32)
        s4 = P.sbuf("s4C", [128, 4, 144], F32)
        s8 = P.sbuf("s8C", [128, 4, 144], F32)
        s16 = P.sbuf("s16C", [128, 4, 144], F32)
        avg = P.sbuf("avgC", [128, 4, 128], F32)
        mixs = [P.sbuf("mixC%d" % i, [128, 4, 128], BF16) for i in range(2)]
        aTs = [P.sbuf("aTC%d" % i, [128, 4, 128], BF16) for i in range(2)]
        boTs = [P.sbuf("boTC%d" % i, [128, 4, 128], BF16) for i in range(2)]
        sig0s = [P.sbuf("sig0C%d" % i, [128, D], F32) for i in range(2)]
        sig1s = [P.sbuf("sig1C%d" % i, [128, D], F32) for i in range(2)]
        m0 = P.sbuf("m0C", [128, D], F32)
        m1s = [P.sbuf("m1C%d" % i, [128, D], F32) for i in range(2)]
        mgs = [P.sbuf("mgC%d" % i, [128, D], BF16) for i in range(2)]
        mgTs = [P.sbuf("mgTC%d" % i, [128, 8, 128], BF16) for i in range(2)]
        hms = [P.sbuf("hmC%d" % i, [128, D], F32) for i in range(2)]
        pU = [P.psum("pUC%d" % i, [128, 512], F32) for i in range(2)]
        pY = P.psum("pYC", [128, 512], F32)
        pG = P.psum("pGC", [128, 1024], F32)
        pX = P.psum("pXC", [128, 1024], F32)
        pTr = P.psum("pTrC", [128, 512], F32)

        P.dma("sp", id32[:], ident_in, writes=["id32C"])
        P.add("dve", lambda e: e.tensor_copy(out=idb[:], in_=id32[:]), reads=["id32C"], writes=["idbC"])
        for g in range(4):
            P.dma("sp", psc[:, g:g + 1], pool_scale[L, g * 128:(g + 1) * 128].rearrange("(p o) -> p o", o=1), writes=["pscC"], partial=True)
        P.dma("sp", inv[:].rearrange("p a g t -> p (a g t)"), invc_in.broadcast_to([128, 1024]), writes=["invC"])
        P.add("pool", lambda e: e.memset(zt[:], 0.0), writes=["ztC"])
        load_cast(P, wu_, w_in[L][:, 0:512], 512, stg, "wuC")
        load_cast(P, wgt, w_in[L][:, 2048:4096], 2048, stg, "wgtC")
        load_cast(P, wpu, w_pu[L], D, stg, "wpuC", nchunk=4)
        load_cast(P, wau, w_au[L], D, stg, "wauC", nchunk=4)
        load_cast(P, wo, w_out[L], D, stg, "woC")
        sv = stg[0][:, 0:512].rearrange("p (g d) -> p g d", d=128)
        P.dma("sp", sv, pool_gw[L].rearrange("g c d -> c g d"), writes=[stg[0].name])
        P.add("dve", lambda e: e.tensor_copy(out=gw[:], in_=sv), reads=[stg[0].name], writes=["gwC"])
        if L == 1:
            for jb in range(NB, 2 * NBH):
                P.dma("sp", HM[jb * 128:(jb + 1) * 128, :], zt[:], reads=["ztC"], writes=["HM"], partial=True)
        def issue_loads(j):
            s = j % 2
            x_, h, bo = hx[s], hts[j % 3], bos[s]
            if j == 0:
                P.add("pool", lambda e, x_=x_: e.memset(x_[:, :, 0:16], 0.0), writes=[x_.name])
                P.dma("sp", x_[:, :, 16:144], HNT[:, :, 0:128], writes=[x_.name])
            else:
                P.dma("sp", x_[:], HNT[:, :, j * 128 - 16:(j + 1) * 128], writes=[x_.name])
            P.dma("sp", h[:], src[j * 128:(j + 1) * 128, :], writes=[h.name])
            P.dma("sp", bo[:], BO[j * 128:(j + 1) * 128, :], writes=[bo.name])

        ptr_b = pTr[:].bitcast(BF16)

        def slot(j):
            s = j % 2
            return dict(x_=hx[s], h=hts[j % 3], bo=bos[s], hm=hms[s], u32=u32s[s], mix=mixs[s], aT=aTs[s], boT=boTs[s], sig0=sig0s[s],
                        sig1=sig1s[s], m1=m1s[s], mg=mgs[s], mgT=mgTs[s])

        def gmm(e, x_, off):
            for n in range(2):
                for k in range(8):
                    ins = e.matmul(pG[:, n * 512:(n + 1) * 512], lhsT=x_[:, k, 16:144], rhs=wgt[:, k, off + n * 512:off + (n + 1) * 512],
                                   start=(k == 0), stop=(k == 7))
            return ins

        def e_umm(j):
            t = slot(j)
            x_, u32 = t["x_"], t["u32"]
            for half in range(2):
                pu = pU[half]

                def umm(e, pu=pu, half=half):
                    for gg in range(2):
                        g = half * 2 + gg
                        for k in range(8):
                            ins = e.matmul(pu[:, gg * 144:(gg + 1) * 144], lhsT=wu_[:, k, g * 128:(g + 1) * 128], rhs=x_[:, k, :],
                                           start=(k == 0), stop=(k == 7))
                    return ins
                P.add("pe", umm, reads=["wuC", x_.name], writes=[pu.name])
                P.add("act", lambda e, pu=pu, half=half: e.activation(out=u32[:, half * 2:half * 2 + 2, :].rearrange("p g t -> p (g t)"),
                                                                    in_=pu[:, 0:288], func=AF.Copy),
                      reads=[pu.name], writes=[u32.name])

        def e_gate(j, which):
            t = slot(j)
            x_ = t["x_"]
            sg = t["sig0"] if which == 0 else t["sig1"]
            P.add("pe", lambda e: gmm(e, x_, which * 1024), reads=["wgtC", x_.name], writes=["pGC"])
            P.add("act", lambda e: e.activation(out=sg[:], in_=pG[:], func=AF.Sigmoid), reads=["pGC"], writes=[sg.name])

        def e_trb(j):
            t = slot(j)
            bo, boT = t["bo"], t["boT"]

            def trb(e):
                for c in range(4):
                    ins = e.transpose(out=ptr_b[:, c * 128:(c + 1) * 128], in_=bo[:, c * 128:(c + 1) * 128], identity=idb[:])
                return ins
            P.add("pe", trb, reads=[bo.name, "idbC"], writes=["pTrC"])
            P.add("dve", lambda e: e.tensor_copy(out=boT[:].rearrange("p c t -> p (c t)"), in_=ptr_b[:, 0:512]), reads=["pTrC"], writes=[boT.name])

        def e_pool(j):
            t = slot(j)
            u32, mix = t["u32"], t["mix"]
            P.add("dve", lambda e: e.tensor_tensor(out=s2[:, :, 1:144], in0=u32[:, :, 1:144], in1=u32[:, :, 0:143], op=ALU.add),
                  reads=[u32.name], writes=["s2C"])
            P.add("dve", lambda e: e.tensor_tensor(out=s4[:, 1:4, 3:144], in0=s2[:, 1:4, 3:144], in1=s2[:, 1:4, 1:142], op=ALU.add),
                  reads=["s2C"], writes=["s4C"])
            P.add("dve", lambda e: e.tensor_tensor(out=s8[:, 2:4, 7:144], in0=s4[:, 2:4, 7:144], in1=s4[:, 2:4, 3:140], op=ALU.add),
                  reads=["s4C"], writes=["s8C"])
            P.add("dve", lambda e: e.tensor_tensor(out=s16[:, 3:4, 15:144], in0=s8[:, 3:4, 15:144], in1=s8[:, 3:4, 7:136], op=ALU.add),
                  reads=["s8C"], writes=["s16C"])
            iv = 1 if j == 0 else 0
            for g, (st, sn) in enumerate(((s2, "s2C"), (s4, "s4C"), (s8, "s8C"), (s16, "s16C"))):
                P.add("dve", lambda e, g=g, st=st: e.tensor_tensor(out=avg[:, g, :], in0=st[:, g, 16:144], in1=inv[:, iv, g, :], op=ALU.mult),
                      reads=[sn, "invC"], writes=["avgC"])
            P.add("dve", lambda e: e.tensor_tensor(out=mix[:], in0=avg[:], in1=u32[:, :, 16:144], op=ALU.subtract),
                  reads=["avgC", u32.name], writes=[mix.name])

        def l_ymm(j):
            t = slot(j)
            mix, aT = t["mix"], t["aT"]

            def ymm(e):
                for g in range(4):
                    ins = e.matmul(pY[:, g * 128:(g + 1) * 128], lhsT=gw[:, g, :], rhs=mix[:, g, :], start=True, stop=True)
                return ins
            P.add("pe", ymm, reads=["gwC", mix.name], writes=["pYC"])
            for g in range(4):
                P.add("dve", lambda e, g=g: e.tensor_scalar(out=aT[:, g, :], in0=pY[:, g * 128:(g + 1) * 128], scalar1=psc[:, g:g + 1], scalar2=0.0,
                                                            op0=ALU.mult, op1=ALU.add),
                      reads=["pYC", "pscC"], writes=[aT.name])

        def l_aup(j):
            t = slot(j)
            aT, sig0 = t["aT"], t["sig0"]

            def aup(e):
                for n in range(2):
                    for g in range(4):
                        ins = e.matmul(pX[:, n * 512:(n + 1) * 512], lhsT=aT[:, g, :], rhs=wpu[:, g, n * 512:(n + 1) * 512],
                                       start=(g == 0), stop=(g == 3))
                return ins
            P.add("pe", aup, reads=["wpuC", aT.name], writes=["pXC"])
            P.add("dve", lambda e: e.tensor_tensor(out=m0[:], in0=sig0[:], in1=pX[:], op=ALU.mult), reads=[sig0.name, "pXC"], writes=["m0C"])

        def l_bup(j):
            t = slot(j)
            boT, sig1, m1, mg = t["boT"], t["sig1"], t["m1"], t["mg"]

            def bup(e):
                for n in range(2):
                    for g in range(4):
                        ins = e.matmul(pX[:, n * 512:(n + 1) * 512], lhsT=boT[:, g, :], rhs=wau[:, g, n * 512:(n + 1) * 512],
                                       start=(g == 0), stop=(g == 3))
                return ins
            P.add("pe", bup, reads=["wauC", boT.name], writes=["pXC"])
            P.add("dve", lambda e: e.tensor_tensor(out=m1[:], in0=sig1[:], in1=pX[:], op=ALU.mult), reads=[sig1.name, "pXC"], writes=[m1.name])
            P.add("dve", lambda e: e.tensor_tensor(out=mg[:], in0=m0[:], in1=m1[:], op=ALU.add), reads=["m0C", m1.name], writes=[mg.name])

        def l_trm(j):
            t = slot(j)
            mg, mgT = t["mg"], t["mgT"]

            def trm(e):
                for c in range(8):
                    ins = e.transpose(out=ptr_b[:, c * 128:(c + 1) * 128], in_=mg[:, c * 128:(c + 1) * 128], identity=idb[:])
                return ins
            P.add("pe", trm, reads=[mg.name, "idbC"], writes=["pTrC"])
            P.add("act", lambda e: e.activation(out=mgT[:].rearrange("p c t -> p (c t)"), in_=ptr_b, func=AF.Copy),
                  reads=["pTrC"], writes=[mgT.name])

        def l_omm(j):
            t = slot(j)
            mgT, h, hm = t["mgT"], t["h"], t["hm"]

            def omm(e):
                for n in range(2):
                    for k in range(8):
                        ins = e.matmul(pX[:, n * 512:(n + 1) * 512], lhsT=mgT[:, k, :], rhs=wo[:, k, n * 512:(n + 1) * 512],
                                       start=(k == 0), stop=(k == 7))
                return ins
            P.add("pe", omm, reads=["woC", mgT.name], writes=["pXC"])
            P.add("dve", lambda e: e.tensor_tensor(out=hm[:], in0=pX[:], in1=h[:], op=ALU.add), reads=["pXC", h.name], writes=[hm.name])
            P.dma("sp", HM[j * 128:(j + 1) * 128, :], hm[:], reads=[hm.name], writes=["HM"], partial=True)

        issue_loads(0)
        for j in range(NB + 1):
            je, jl = (j if j < NB else None), (j - 1 if j >= 1 else None)
            if je is not None and je + 1 < NB:
                issue_loads(je + 1)
            if je is not None:
                e_umm(je)
            if jl is not None:
                l_ymm(jl)
            if je is not None:
                e_gate(je, 0)
            if jl is not None:
                l_aup(jl)
            if je is not None:
                e_trb(je)
            if jl is not None:
                l_bup(jl)
            if je is not None:
                e_pool(je)
                e_gate(je, 1)
            if jl is not None:
                l_trm(jl)
                l_omm(jl)
        P.emit()

    def phase_d(L):
        moe = (L == 1)
        nblk = NBH if moe else NB
        GMAX = 11
        ngr = (nblk + GMAX - 1) // GMAX
        base = nblk // ngr
        rem = nblk % ngr
        gsz = [base + (1 if i < rem else 0) for i in range(ngr)]
        GM = max(gsz)
        nexp = NEXP if moe else 1
        FC = 256
        nfc = DFF // FC
        blk0 = 0
        for gi, G in enumerate(gsz):
            P = Prog(nc)
            acc = P.sbuf("accD", [128, GM, D], F32)
            hnT = P.sbuf("hnTD", [128, 8, GM * 128], BF16)
            g_t = P.sbuf("gD", [128, D], F32)
            eps_t = P.sbuf("epsD", [128, 1], F32)
            id32 = P.sbuf("id32D", [128, 128], F32)
            idb = P.sbuf("idbD", [128, 128], BF16)
            pvt = P.sbuf("pvD", [128, 2], F32)
            hb = [P.sbuf("hbD%d" % i, [128, D], F32) for i in range(2)]
            junk = P.sbuf("junkD", [128, D], BF16)
            sss = [P.sbuf("ssD%d" % i, [128, 1], F32) for i in range(2)]
            rss = [P.sbuf("rsD%d" % i, [128, 1], F32) for i in range(2)]
            hns = [P.sbuf("hnD%d" % i, [128, D], BF16) for i in range(2)]
            sgw = [P.sbuf("sgwD%d" % i, [128, 8, FC], F32) for i in range(2)]
            suw = [P.sbuf("suwD%d" % i, [128, 8, FC], F32) for i in range(2)]
            sdw = [P.sbuf("sdwD%d" % i, [128, 2, D], F32) for i in range(2)]
            wgb = [P.sbuf("wgbD%d" % i, [128, 8, FC], BF16) for i in range(2)]
            wub = [P.sbuf("wubD%d" % i, [128, 8, FC], BF16) for i in range(2)]
            wdb = [P.sbuf("wdbD%d" % i, [128, 2, D], BF16) for i in range(2)]
            sgt = [P.sbuf("sgtD%d" % i, [128, 256], F32) for i in range(4)]
            actT = [P.sbuf("actD%d" % i, [128, 2, 256], BF16) for i in range(2)]
            pGU = [P.psum("pGUD%d" % i, [128, 2, 256], F32) for i in range(4)]
            pDN = [P.psum("pDND%d" % i, [128, 1024], F32) for i in range(2)]
            pTr_t, pR_t = pGU[2], pGU[3]
            pTr = pTr_t[:].rearrange("p a b -> p (a b)")
            pR = pR_t[:].rearrange("p a b -> p (a b)")
            if moe:
                rt32 = P.sbuf("rt32D", [128, 8, NEXP], F32)
                rtb = P.sbuf("rtbD", [128, 8, NEXP], BF16)
                cw = P.sbuf("cwD", [128, GM, NEXP], F32)
                lg = P.sbuf("lgD", [128, NEXP], F32)
                lg2 = P.sbuf("lg2D", [128, NEXP], F32)
                eq = P.sbuf("eqD", [128, NEXP], F32)
                ex = P.sbuf("exD", [128, NEXP], F32)
                mm1 = P.sbuf("mm1D", [128, 1], F32)
                mm2 = P.sbuf("mm2D", [128, 1], F32)
                sm = P.sbuf("smD", [128, 1], F32)
            if L == 1:
                gf = P.sbuf("gfD", [128, D], F32)
                P.dma("sp", gf[:], final_norm.broadcast_to([128, D]), writes=["gfD"])
            P.dma("sp", g_t[:], norm_ffn[L:L + 1, :].broadcast_to([128, D]), writes=["gD"])
            P.add("pool", lambda e: e.memset(eps_t[:], 1e-6), writes=["epsD"])
            P.dma("sp", id32[:], ident_in, writes=["id32D"])
            P.add("dve", lambda e: e.tensor_copy(out=idb[:], in_=id32[:]), reads=["id32D"], writes=["idbD"])
            P.dma("sp", pvt[:], pv_in, writes=["pvD"])
            if moe:
                P.dma("sp", rt32[:], router[0].rearrange("(c p) n -> p c n", p=128), writes=["rt32D"])
                P.add("dve", lambda e: e.tensor_copy(out=rtb[:], in_=rt32[:]), reads=["rt32D"], writes=["rtbD"])
            for i in range(G):
                jb = blk0 + i
                s = i % 2
                a_ap = acc[:, i, :]
                ares = "acc%d" % i
                if moe:
                    hA, hB = hb[0], hb[1]
                    P.dma("sp", hA[:], HM[jb * 128:(jb + 1) * 128, :], writes=[hA.name])
                    P.dma("sp", hB[:], HM[(NBH + jb) * 128:(NBH + jb + 1) * 128, :], writes=[hB.name])
                    P.add("dve", lambda e, hB=hB: e.tensor_scalar(out=hB[:], in0=hB[:], scalar1=pvt[:, 0:1], scalar2=0.0, op0=ALU.mult, op1=ALU.add),
                          reads=[hB.name, "pvD"], writes=[hB.name])
                    P.add("dve", lambda e, hA=hA, hB=hB, a_ap=a_ap: e.scalar_tensor_tensor(out=a_ap, in0=hA[:], scalar=pvt[:, 1:2], in1=hB[:],
                                                                                         op0=ALU.mult, op1=ALU.add),
                          reads=[hA.name, hB.name, "pvD"], writes=[ares])
                else:
                    P.dma("sp", a_ap, HM[jb * 128:(jb + 1) * 128, :], writes=[ares])
                ss, rs, hn = sss[s], rss[s], hns[s]
                rmsnorm_block(P, a_ap, ares, g_t, eps_t, junk, ss, rs, hn, hn.name)
                ptr_b = pTr[:].bitcast(BF16)

                def tr(e, hn=hn, ptr_b=ptr_b):
                    for c in range(8):
                        ins = e.transpose(out=ptr_b[:, c * 128:(c + 1) * 128], in_=hn[:, c * 128:(c + 1) * 128], identity=idb[:])
                    return ins
                P.add("pe", tr, reads=[hn.name, "idbD"], writes=[pTr_t.name])
                P.add("act", lambda e, i=i, ptr_b=ptr_b: e.activation(out=hnT[:, :, i * 128:(i + 1) * 128],
                                                                    in_=ptr_b.rearrange("p (c t) -> p c t", t=128), func=AF.Copy),
                      reads=[pTr_t.name], writes=["hnT%d" % i])
                if moe:
                    def rmm(e, i=i):
                        for k in range(8):
                            ins = e.matmul(pR[:, 0:NEXP], lhsT=hnT[:, k, i * 128:(i + 1) * 128], rhs=rtb[:, k, :], start=(k == 0), stop=(k == 7))
                        return ins
                    P.add("pe", rmm, reads=["rtbD", "hnT%d" % i], writes=[pR_t.name])
                    P.add("dve", lambda e: e.tensor_copy(out=lg[:], in_=pR[:, 0:NEXP]), reads=[pR_t.name], writes=["lgD"])
                    P.add("dve", lambda e: e.reduce_max(out=mm1[:], in_=lg[:], axis=AX.X), reads=["lgD"], writes=["mm1D"])
                    P.add("dve", lambda e: e.tensor_scalar(out=eq[:], in0=lg[:], scalar1=mm1[:, 0:1], scalar2=0.0, op0=ALU.is_equal, op1=ALU.add),
                          reads=["lgD", "mm1D"], writes=["eqD"])
                    P.add("dve", lambda e: e.scalar_tensor_tensor(out=lg2[:], in0=eq[:], scalar=-1e30, in1=lg[:], op0=ALU.mult, op1=ALU.add),
                          reads=["eqD", "lgD"], writes=["lg2D"])
                    P.add("dve", lambda e: e.reduce_max(out=mm2[:], in_=lg2[:], axis=AX.X), reads=["lg2D"], writes=["mm2D"])
                    P.add("dve", lambda e: e.tensor_scalar(out=eq[:], in0=lg[:], scalar1=mm2[:, 0:1], scalar2=0.0, op0=ALU.is_ge, op1=ALU.add),
                          reads=["lgD", "mm2D", "eqD"], writes=["eqD"])
                    P.add("dve", lambda e: e.tensor_scalar(out=mm1[:], in0=mm1[:], scalar1=-1.0, scalar2=0.0, op0=ALU.mult, op1=ALU.add),
                          reads=["mm1D"], writes=["mm1D"])
                    P.add("act", lambda e: e.activation(out=ex[:], in_=lg[:], func=AF.Exp, bias=mm1[:], scale=1.0),
                          reads=["lgD", "mm1D"], writes=["exD"])
                    P.add("dve", lambda e: e.tensor_tensor(out=ex[:], in0=ex[:], in1=eq[:], op=ALU.mult), reads=["exD", "eqD"], writes=["exD"])
                    P.add("dve", lambda e: e.reduce_sum(out=sm[:], in_=ex[:], axis=AX.X), reads=["exD"], writes=["smD"])
                    P.add("dve", lambda e: e.reciprocal(out=sm[:], in_=sm[:]), reads=["smD"], writes=["smD"])
                    P.add("dve", lambda e, i=i: e.tensor_scalar(out=cw[:, i, :], in0=ex[:], scalar1=sm[:, 0:1], scalar2=0.0, op0=ALU.mult, op1=ALU.add),
                          reads=["exD", "smD"], writes=["cw%d" % i])
            tiles = [(t0, min(2, G - t0)) for t0 in range(0, G, 2)]
            wi = 0
            gi_ = 0
            ti_ = 0
            pend = [None]
            for ex_i in range(nexp):
                if moe:
                    Wg, Wu, Wd = mwg[0][ex_i], mwu[0][ex_i], mwd[0][ex_i]
                else:
                    Wg, Wu, Wd = dwg[0], dwu[0], dwd[0]
                Wgv = Wg.rearrange("(c p) n -> p c n", p=128)
                Wuv = Wu.rearrange("(c p) n -> p c n", p=128)
                Wdv = Wd.rearrange("(c p) n -> p c n", p=128)
                for fc in range(nfc):
                    ws = wi % 2
                    wi += 1
                    f0 = fc * FC
                    P.dma("sp", sgw[ws][:], Wgv[:, :, f0:f0 + FC], writes=[sgw[ws].name])
                    P.dma("sp", suw[ws][:], Wuv[:, :, f0:f0 + FC], writes=[suw[ws].name])
                    P.dma("sp", sdw[ws][:], Wdv[:, fc * 2:fc * 2 + 2, :], writes=[sdw[ws].name])
                    P.add("pool", lambda e, ws=ws: e.tensor_copy(out=wgb[ws][:], in_=sgw[ws][:]), reads=[sgw[ws].name], writes=[wgb[ws].name])
                    P.add("pool", lambda e, ws=ws: e.tensor_copy(out=wub[ws][:], in_=suw[ws][:]), reads=[suw[ws].name], writes=[wub[ws].name])
                    P.add("act", lambda e, ws=ws: e.activation(out=wdb[ws][:], in_=sdw[ws][:], func=AF.Copy), reads=[sdw[ws].name], writes=[wdb[ws].name])
                    for (t0, nb_) in tiles:
                        W = nb_ * 128
                        at = actT[ti_ % 2]
                        ti_ += 1
                        for fs in range(2):
                            gu = pGU[gi_ % 4]
                            sg = sgt[gi_ % 4]
                            gi_ += 1

                            def gumm(e, gu=gu, ws=ws, fs=fs, t0=t0, W=W):
                                for which, wb in ((0, wgb[ws]), (1, wub[ws])):
                                    for k in range(8):
                                        ins = e.matmul(gu[:, which, 0:W], lhsT=wb[:, k, fs * 128:(fs + 1) * 128],
                                                       rhs=hnT[:, k, t0 * 128:t0 * 128 + W], start=(k == 0), stop=(k == 7))
                                return ins
                            P.add("pe", gumm, reads=[wgb[ws].name, wub[ws].name] + ["hnT%d" % (t0 + b) for b in range(nb_)], writes=[gu.name])
                            P.add("act", lambda e, gu=gu, sg=sg, W=W: e.activation(out=sg[:, 0:W], in_=gu[:, 0, 0:W], func=AF.Silu),
                                  reads=[gu.name], writes=[sg.name])
                            P.add("dve", lambda e, gu=gu, sg=sg, at=at, fs=fs, W=W: e.tensor_tensor(out=at[:, fs, 0:W], in0=sg[:, 0:W], in1=gu[:, 1, 0:W], op=ALU.mult),
                                  reads=[gu.name, sg.name], writes=[at.name + "_%d" % fs])

                        def emit_dn(at=at, ws=ws, t0=t0, nb_=nb_, ex_i=ex_i):
                            for b in range(nb_):
                                dn = pDN[b]
                                i = t0 + b

                                def dmm(e, dn=dn, b=b):
                                    for n in range(2):
                                        for fs in range(2):
                                            ins = e.matmul(dn[:, n * 512:(n + 1) * 512], lhsT=at[:, fs, b * 128:(b + 1) * 128],
                                                           rhs=wdb[ws][:, fs, n * 512:(n + 1) * 512], start=(fs == 0), stop=(fs == 1))
                                    return ins
                                P.add("pe", dmm, reads=[at.name + "_0", at.name + "_1", wdb[ws].name], writes=[dn.name])
                                if moe:
                                    P.add("dve", lambda e, dn=dn, i=i: e.scalar_tensor_tensor(out=acc[:, i, :], in0=dn[:], scalar=cw[:, i, ex_i:ex_i + 1],
                                                                                                in1=acc[:, i, :], op0=ALU.mult, op1=ALU.add),
                                          reads=[dn.name, "cw%d" % i, "acc%d" % i], writes=["acc%d" % i])
                                else:
                                    P.add("dve", lambda e, dn=dn, i=i: e.tensor_tensor(out=acc[:, i, :], in0=dn[:], in1=acc[:, i, :], op=ALU.add),
                                          reads=[dn.name, "acc%d" % i], writes=["acc%d" % i])
                        if pend[0] is not None:
                            pend[0]()
                        pend[0] = emit_dn
            if pend[0] is not None:
                pend[0]()
                pend[0] = None
            for i in range(G):
                jb = blk0 + i
                if L == 0:
                    P.dma("sp", H1[jb * 128:(jb + 1) * 128, :], acc[:, i, :], reads=["acc%d" % i], writes=["H1"], partial=True)
                else:
                    s = i % 2
                    ss, rs, ho = sss[s], rss[s], hb[s]
                    a_ap = acc[:, i, :]
                    ares = "acc%d" % i
                    P.add("act", lambda e, a_ap=a_ap, ss=ss: e.activation(out=junk[:], in_=a_ap, func=AF.Square, accum_out=ss[:]),
                          reads=[ares], writes=["junkD", ss.name])
                    P.add("act", lambda e, ss=ss, rs=rs: e.activation(out=rs[:], in_=ss[:], func=AF.Sqrt, bias=eps_t[:], scale=1.0 / D),
                          reads=[ss.name], writes=[rs.name])
                    P.add("dve", lambda e, rs=rs: e.reciprocal(out=rs[:], in_=rs[:]), reads=[rs.name], writes=[rs.name])
                    P.add("dve", lambda e, a_ap=a_ap, rs=rs, ho=ho: e.scalar_tensor_tensor(out=ho[:], in0=a_ap, scalar=rs[:, 0:1], in1=gf[:],
                                                                                         op0=ALU.mult, op1=ALU.mult),
                          reads=[ares, rs.name, "gfD"], writes=[ho.name])
                    P.dma("sp", out[jb * 128:(jb + 1) * 128, :], ho[:], reads=[ho.name], writes=["out"], partial=True)
            P.emit()
            blk0 += G

    plist = [("S", 0)] + [(ph, L) for L in range(2) for ph in "ABCD"]
    if phases is not None:
        plist = plist[:phases]
    for ph, L in plist:
        {"S": lambda L: phase_setup(), "A": phase_a, "B": phase_b, "C": phase_c, "D": phase_d}[ph](L)
    return nc


_IN_NAMES = ["rel_bias", "norm_mix", "w_in", "pool_group_w", "pool_scale", "lambda_q1", "lambda_k1", "lambda_q2",
             "lambda_k2", "subln_gain", "w_pool_up", "w_attn_up", "w_out", "norm_ffn", "dense_w_gate", "dense_w_up",
             "dense_w_down", "moe_router", "moe_w_gate", "moe_w_up", "moe_w_down"]


def run(inputs, dbg=False, phases=None):
    x = np.asarray(inputs["x"], np.float32)
    B, S, _ = x.shape
    meta = np.asarray(inputs["meta_tokens"], np.float32)
    Ltot = 16 + S
    NB = (Ltot + 127) // 128
    NBH = (NB + 2) // 2 if NB % 2 == 1 else NB // 2 + 1
    NBH = (NB + 1) // 2
    T = NB * 128
    ncores = 2 * B
    oh, invc = _const_tables()
    ident = np.eye(128, dtype=np.float32)
    shared = {k: np.ascontiguousarray(np.asarray(inputs[k], np.float32)) for k in _IN_NAMES}
    shared["final_norm"] = np.asarray(inputs["final_norm"], np.float32).reshape(1, D)
    shared["ident"] = ident
    shared["ohtab"] = oh
    shared["invcnt"] = invc
    in_maps = []
    for c in range(ncores):
        b, p = c // 2, c % 2
        xin = np.zeros((T, D), np.float32)
        xin[0:16] = meta
        xin[16:Ltot] = x[b]
        pv = np.zeros((128, 2), np.float32)
        pv[:, 0] = p
        pv[:, 1] = 1 - p
        m = dict(shared)
        m["xin"] = xin
        m["pvec"] = pv
        in_maps.append(m)
    nc = build(NB, NBH, dbg=dbg, phases=phases)
    res = run_bass_kernel_spmd(nc, in_maps, core_ids=list(range(ncores)))
    outs = []
    for b in range(B):
        full = np.concatenate([res.results[2 * b]["out"], res.results[2 * b + 1]["out"]], axis=0)
        outs.append(full[16:Ltot])
    o = np.stack(outs, axis=0).astype(np.float32)
    if dbg:
        return o, res
    return o


def kernel(**inputs):
    return run(inputs)
```

```python
import math
import numpy as np
import concourse.bass as bass
import concourse.mybir as mybir
from concourse.bass_utils import run_bass_kernel_spmd
from contextlib import ExitStack

F32 = mybir.dt.float32
BF16 = mybir.dt.bfloat16
AF = mybir.ActivationFunctionType
ALU = mybir.AluOpType
AX = mybir.AxisListType

ENGS = ("pe", "act", "dve", "pool", "sp")
NDMA_SEMS = 12
SELF_SYNC = ("act", "dve", "pool")


class Prog:
    def __init__(self, nc):
        self.nc = nc
        self.ops = []
        self.last_w = {}
        self.readers = {}
        self.es = ExitStack()

    _n = [0]
    G = None

    def sbuf(self, name, shape, dt):
        Prog._n[0] += 1
        return self.es.enter_context(self.nc.sbuf_tensor("%s_u%d" % (name, Prog._n[0]), shape, dt))

    def psum(self, name, shape, dt):
        Prog._n[0] += 1
        return self.es.enter_context(self.nc.psum_tensor("%s_u%d" % (name, Prog._n[0]), shape, dt))

    def add(self, eng, fn, reads=(), writes=(), dma=False, partial=False):
        idx = len(self.ops)
        deps = set()
        for r in reads:
            for w in self.last_w.get(r, ()):
                deps.add(w)
        for w in writes:
            for pw in self.last_w.get(w, ()):
                if not (partial and self.ops[pw]["partial"]):
                    deps.add(pw)
            for rd in self.readers.get(w, ()):
                deps.add(rd)
        deps.discard(idx)
        self.ops.append(dict(eng=eng, fn=fn, deps=deps, dma=dma, idx=idx, partial=partial))
        for r in reads:
            self.readers.setdefault(r, []).append(idx)
        for w in writes:
            if partial and not self.readers.get(w):
                self.last_w.setdefault(w, []).append(idx)
            else:
                self.last_w[w] = [idx]
            self.readers[w] = []
        return idx

    def dma(self, q, out, in_, reads=(), writes=(), partial=False, **kw):
        return self.add(q, lambda e: e.dma_start(out=out, in_=in_, **kw), reads, writes, dma=True, partial=partial)

    def emit(self):
        nc = self.nc
        ops = self.ops
        need = [False] * len(ops)
        for o in ops:
            for d in o["deps"]:
                po = ops[d]
                if po["dma"] or po["eng"] != o["eng"] or po["eng"] in SELF_SYNC:
                    need[d] = True
        dmaq = set(o["eng"] for o in ops if o["dma"])
        if Prog.G is None or Prog.G["nc"] is not nc:
            ges = ExitStack()
            Prog.G = dict(
                nc=nc, es=ges,
                esem={e: ges.enter_context(nc.semaphore("gs_" + e)) for e in ENGS},
                dsem={e: [ges.enter_context(nc.semaphore("gd_%s%d" % (e, i))) for i in range(NDMA_SEMS)] for e in ("sp",)},
                ecount={e: 0 for e in ENGS},
                dcount={e: [0] * NDMA_SEMS for e in ENGS},
                dn={e: 0 for e in ENGS})
        G = Prog.G
        esem, dsem, ecount, dcount, dn = G["esem"], G["dsem"], G["ecount"], G["dcount"], G["dn"]
        for o in ops:
            i = o["idx"]
            e = o["eng"]
            o["sig"] = None
            o["prewait"] = None
            if o["dma"]:
                k = dn[e] % NDMA_SEMS
                dn[e] += 1
                if dcount[e][k] > 0:
                    o["prewait"] = (dsem[e][k], dcount[e][k], ("d", e, k))
                dcount[e][k] += 16
                o["sig"] = (dsem[e][k], dcount[e][k], ("d", e, k), 16)
            elif need[i]:
                ecount[e] += 1
                o["sig"] = (esem[e], ecount[e], ("e", e), 1)
        streams = {e: [o for o in ops if o["eng"] == e] for e in ENGS}
        final_waits = {e: [] for e in ENGS}
        for e in dmaq:
            for k in range(NDMA_SEMS):
                if dcount[e][k] > 0:
                    final_waits[e].append((dsem[e][k], dcount[e][k]))

        def run_stream(e, eng):
            waited = {}
            for o in streams[e]:
                ws = []
                if o["prewait"] is not None:
                    ws.append(o["prewait"])
                for d in sorted(o["deps"]):
                    po = ops[d]
                    if po["dma"] or po["eng"] != e or e in SELF_SYNC:
                        s = po["sig"]
                        ws.append((s[0], s[1], s[2]))
                best = {}
                for (sem, val, key) in ws:
                    if key not in best or best[key][1] < val:
                        best[key] = (sem, val)
                for key, (sem, val) in best.items():
                    if waited.get(key, 0) >= val:
                        continue
                    eng.wait_ge(sem, val)
                    waited[key] = val
                ins = o["fn"](eng)
                if o["sig"] is not None:
                    ins.then_inc(o["sig"][0], o["sig"][3])
            for (sem, val) in final_waits[e]:
                eng.wait_ge(sem, val)

        with nc.Block() as block:
            @block.tensor
            def _(eng):
                run_stream("pe", eng)

            @block.scalar
            def _(eng):
                run_stream("act", eng)

            @block.vector
            def _(eng):
                run_stream("dve", eng)

            @block.gpsimd
            def _(eng):
                run_stream("pool", eng)

            @block.sync
            def _(eng):
                run_stream("sp", eng)
        self.es.close()


D = 1024
DFF = 3584
NEXP = 8
LAMBDA_INIT = [0.8 - 0.6 * math.exp(-0.3 * l) for l in range(2)]
NEG = -30000.0


def _bucket(n):
    n = np.maximum(n, 0)
    nf = np.maximum(n, 1).astype(np.float32)
    large = 16 + (np.log(nf / np.float32(16)) / np.float32(math.log(8.0)) * np.float32(16)).astype(np.int32)
    large = np.minimum(large, 31)
    return np.where(n < 16, n, large)


def _const_tables():
    k = np.arange(128)[:, None]
    q = np.arange(128)[None, :]
    oh = np.zeros((33, 2, 128, 128), np.float32)
    for v in range(2):
        dist = q - k + 128 * v
        bk = _bucket(dist)
        valid = dist >= 0
        for b in range(32):
            oh[b, v] = ((bk == b) & valid).astype(np.float32)
        oh[32, v] = np.where(valid, 0.0, NEG)
    invc = np.zeros((2, 4, 128), np.float32)
    for g, w in enumerate((2, 4, 8, 16)):
        invc[0, g, :] = 1.0 / w
        invc[1, g, :] = 1.0 / np.minimum(np.arange(128) + 1, w)
    return oh.reshape(33, 2 * 128 * 128), invc.reshape(1, 2 * 4 * 128)


def build(NB, NBH, dbg=False, phases=None):
    T = NB * 128
    nc = bass.Bass("TRN2", target_bir_lowering=False)

    def din(name, shape):
        return nc.dram_tensor(name, shape, F32, kind="ExternalInput").ap()

    xin = din("xin", [T, D])
    pv_in = din("pvec", [128, 2])
    ident_in = din("ident", [128, 128])
    oh_in = din("ohtab", [33, 2 * 128 * 128])
    invc_in = din("invcnt", [1, 2 * 4 * 128])
    rel_bias = din("rel_bias", [32, 4])
    norm_mix = din("norm_mix", [2, D])
    w_in = din("w_in", [2, D, 4096])
    pool_gw = din("pool_group_w", [2, 4, 128, 128])
    pool_scale = din("pool_scale", [2, 512])
    lq1 = din("lambda_q1", [2, 64])
    lk1 = din("lambda_k1", [2, 64])
    lq2 = din("lambda_q2", [2, 64])
    lk2 = din("lambda_k2", [2, 64])
    subln = din("subln_gain", [2, 128])
    w_pu = din("w_pool_up", [2, 512, D])
    w_au = din("w_attn_up", [2, 512, D])
    w_out = din("w_out", [2, D, D])
    norm_ffn = din("norm_ffn", [2, D])
    dwg = din("dense_w_gate", [1, D, DFF])
    dwu = din("dense_w_up", [1, D, DFF])
    dwd = din("dense_w_down", [1, DFF, D])
    router = din("moe_router", [1, D, NEXP])
    mwg = din("moe_w_gate", [1, NEXP, D, DFF])
    mwu = din("moe_w_up", [1, NEXP, D, DFF])
    mwd = din("moe_w_down", [1, NEXP, DFF, D])
    final_norm = din("final_norm", [1, D])
    out = nc.dram_tensor("out", [NBH * 128, D], F32, kind="ExternalOutput").ap()

    def dscr(name, shape, dt):
        kind = "ExternalOutput" if dbg else "Internal"
        return nc.dram_tensor(name, shape, dt, kind=kind).ap()

    NG = (NB + 3) // 4
    NI = (NG + 1) // 2
    NBP = 8 * NI
    TP = NBP * 128
    NS = 4 * NI
    SL = [(i, qi) for i in range(NI) for qi in range(4) if 8 * i + qi < NB]
    assert len(SL) == NBH
    HNT = dscr("s_hnt", [128, 8, TP], BF16)
    KT = dscr("s_kt", [4, 128, TP], BF16)
    QT = dscr("s_qt", [4, 128, TP], BF16)
    VV = dscr("s_vv", [TP, 512], BF16)
    BO = dscr("s_bo", [T, 512], BF16)
    BO1 = dscr("s_bo1", [NS * 128, 512], BF16)
    HM = dscr("s_hm", [T, D], F32)
    HMO = dscr("s_hmo", [NS * 128, D], F32)
    H1 = dscr("s_h1", [TP, D], F32)
    BT = dscr("s_bt", [4, 2 * 128 * 128], F32)
    DBG = dscr("s_dbg", [4, NB, 128, 132], F32) if dbg else None
    DBG2 = dscr("s_dbg2", [128, 384], F32) if dbg else None

    cnt = [0]

    def uid(s):
        cnt[0] += 1
        return "%s_%d" % (s, cnt[0])

    def load_cast(P, dst, src, ncols, stg, tag, nchunk=8):
        srcv = src.rearrange("(c p) n -> p c n", p=128)
        step = max(1, 2048 // ncols)
        i = 0
        for c0 in range(0, nchunk, step):
            c1 = min(nchunk, c0 + step)
            s = stg[i % len(stg)]
            i += 1
            sv = s[:, 0:(c1 - c0) * ncols].rearrange("p (c n) -> p c n", n=ncols)
            P.dma("sp", sv, srcv[:, c0:c1, :], writes=[s.name])
            eng = "pool" if (i % 2 == 0) else "dve"
            P.add(eng, lambda e, a=dst[:, c0:c1, :], b=sv: e.tensor_copy(out=a, in_=b),
                  reads=[s.name], writes=[tag], partial=True)

    def rmsnorm_block(P, h_ap, hres, g_t, eps_t, junk, ss, rstd, hn, hnres, eps_scale=1.0 / D):
        P.add("act", lambda e: e.activation(out=junk[:], in_=h_ap, func=AF.Square, accum_out=ss[:]),
              reads=[hres], writes=[junk.name, ss.name])
        P.add("act", lambda e: e.activation(out=rstd[:], in_=ss[:], func=AF.Sqrt, bias=eps_t[:], scale=eps_scale),
              reads=[ss.name], writes=[rstd.name])
        P.add("dve", lambda e: e.reciprocal(out=rstd[:], in_=rstd[:]), reads=[rstd.name], writes=[rstd.name])
        P.add("dve", lambda e: e.scalar_tensor_tensor(out=hn[:], in0=h_ap, scalar=rstd[:, 0:1], in1=g_t[:],
                                                      op0=ALU.mult, op1=ALU.mult),
              reads=[hres, rstd.name, g_t.name], writes=[hnres])

    def phase_setup():
        P = Prog(nc)
        rb = P.sbuf("rb", [33, 4], F32)
        rb31 = P.sbuf("rb31", [33, 4], F32)
        ohs = [P.sbuf("ohs%d" % i, [33, 4096], F32) for i in range(2)]
        bts = [P.sbuf("bts%d" % i, [4, 512], F32) for i in range(2)]
        ps = [P.psum("sps%d" % i, [128, 512], F32) for i in range(2)]
        P.add("pool", lambda e: e.memset(rb[:], 1.0), writes=["rb"])
        P.dma("sp", rb[0:32, :], rel_bias, writes=["rb"])
        P.dma("sp", rb31[0:32, :], rel_bias[31:32, :].broadcast_to([32, 4]), writes=["rb31"])
        P.add("dve", lambda e: e.tensor_tensor(out=rb[0:32, :], in0=rb[0:32, :], in1=rb31[0:32, :], op=ALU.subtract),
              reads=["rb", "rb31"], writes=["rb"])
        n = 0
        for ch in range(8):
            o = ohs[ch % 2]
            P.dma("sp", o[:], oh_in[:, ch * 4096:(ch + 1) * 4096], writes=[o.name])
            for s in range(8):
                p_ = ps[n % 2]
                b_ = bts[n % 2]
                P.add("pe", lambda e, p_=p_, o=o, s=s: e.matmul(p_[0:4, :], lhsT=rb[:, :], rhs=o[:, s * 512:(s + 1) * 512],
                                                               start=True, stop=True),
                      reads=["rb", o.name], writes=[p_.name])
                P.add("dve", lambda e, p_=p_, b_=b_: e.tensor_copy(out=b_[:], in_=p_[0:4, :]), reads=[p_.name], writes=[b_.name])
                col = ch * 4096 + s * 512
                P.dma("sp", BT[:, col:col + 512], b_[:], reads=[b_.name], writes=["BT"], partial=True)
                n += 1
        P.emit()

    def phase_a(L):
        P = Prog(nc)
        src = xin if L == 0 else H1
        wqkv = P.sbuf("wqkv", [128, 8, 1536], BF16)
        stg = [P.sbuf("stgA%d" % i, [128, 2048], F32) for i in range(2)]
        g_t = P.sbuf("gA", [128, D], F32)
        eps_t = P.sbuf("epsA", [128, 1], F32)
        id32 = P.sbuf("id32A", [128, 128], F32)
        idb = P.sbuf("idbA", [128, 128], BF16)
        hts = [P.sbuf("hA%d" % i, [128, D], F32) for i in range(3)]
        junk = P.sbuf("junkA", [128, D], BF16)
        sss = [P.sbuf("ssA%d" % i, [128, 1], F32) for i in range(2)]
        rss = [P.sbuf("rsA%d" % i, [128, 1], F32) for i in range(2)]
        hns = [P.sbuf("hnA%d" % i, [128, D], BF16) for i in range(2)]
        hnTs = [P.sbuf("hnTA%d" % i, [128, 8, 128], BF16) for i in range(2)]
        kts = [P.sbuf("ktA%d" % i, [128, 4, 128], BF16) for i in range(2)]
        qts = [P.sbuf("qtA%d" % i, [128, 4, 128], BF16) for i in range(2)]
        vts = [P.sbuf("vtA%d" % i, [128, 512], BF16) for i in range(2)]
        pT = [P.psum("pTA%d" % i, [128, 512], F32) for i in range(2)]
        pK = [P.psum("pKA%d" % i, [128, 512], F32) for i in range(2)]
        pQ = [P.psum("pQA%d" % i, [128, 512], F32) for i in range(2)]
        pV = [P.psum("pVA%d" % i, [128, 512], F32) for i in range(2)]

        P.dma("sp", g_t[:], norm_mix[L:L + 1, :].broadcast_to([128, D]), writes=["gA"])
        P.add("pool", lambda e: e.memset(eps_t[:], 1e-6), writes=["epsA"])
        P.dma("sp", id32[:], ident_in, writes=["id32A"])
        P.add("dve", lambda e: e.tensor_copy(out=idb[:], in_=id32[:]), reads=["id32A"], writes=["idbA"])
        load_cast(P, wqkv, w_in[L][:, 512:2048], 1536, stg, "wqkv")
        KTv = KT.rearrange("m p t -> p m t")
        QTv = QT.rearrange("m p t -> p m t")
        if L == 0:
            zbf = P.sbuf("zbfA", [128, 1024], BF16)
            z32 = P.sbuf("z32A", [128, 1024], F32)
            P.add("pool", lambda e: e.memset(zbf[:], 0.0), writes=["zbfA"])
            P.add("pool", lambda e: e.memset(z32[:], 0.0), writes=["z32A"])
            for jb in range(NB, NBP):
                c0, c1 = jb * 128, (jb + 1) * 128
                P.dma("sp", KTv[:, :, c0:c1], zbf[:, 0:512].rearrange("p (m t) -> p m t", t=128), reads=["zbfA"], writes=["KT"], partial=True)
                P.dma("sp", QTv[:, :, c0:c1], zbf[:, 0:512].rearrange("p (m t) -> p m t", t=128), reads=["zbfA"], writes=["QT"], partial=True)
                P.dma("sp", HNT[:, :, c0:c1], zbf[:].rearrange("p (m t) -> p m t", t=128), reads=["zbfA"], writes=["HNT"], partial=True)
                P.dma("sp", VV[c0:c1, :], zbf[:, 0:512], reads=["zbfA"], writes=["VV"], partial=True)
                P.dma("sp", H1[c0:c1, :], z32[:], reads=["z32A"], writes=["H1"], partial=True)
        for jj in range(min(2, NB)):
            P.dma("sp", hts[jj % 3][:], src[jj * 128:(jj + 1) * 128, :], writes=[hts[jj % 3].name])
        for j in range(NB):
            s = j % 2
            h, ss, rs, hn, hnT = hts[j % 3], sss[s], rss[s], hns[s], hnTs[s]
            if j + 2 < NB:
                P.dma("sp", hts[(j + 2) % 3][:], src[(j + 2) * 128:(j + 3) * 128, :], writes=[hts[(j + 2) % 3].name])
            rmsnorm_block(P, h[:], h.name, g_t, eps_t, junk, ss, rs, hn, hn.name)
            pt = pT[s]
            ptb = pt[:].bitcast(BF16)

            def tr(e, hn=hn, ptb=ptb):
                for c in range(8):
                    ins = e.transpose(out=ptb[:, c * 128:(c + 1) * 128], in_=hn[:, c * 128:(c + 1) * 128], identity=idb[:])
                return ins
            P.add("pe", tr, reads=[hn.name, "idbA"], writes=[pt.name])
            P.add("act", lambda e, hnT=hnT, ptb=ptb: e.activation(out=hnT[:].rearrange("p c t -> p (c t)"), in_=ptb, func=AF.Copy),
                  reads=[pt.name], writes=[hnT.name])
            P.dma("sp", HNT[:, :, j * 128:(j + 1) * 128], hnT[:], reads=[hnT.name], writes=["HNT"], partial=True)
            for (pp, tt, off, DR, scale, nm) in ((pQ[s], qts[s], 0, QTv, 0.125, "QT"), (pK[s], kts[s], 512, KTv, 1.0, "KT")):
                def mmf(e, pp=pp, off=off, hnT=hnT):
                    for m in range(4):
                        for k in range(8):
                            ins = e.matmul(pp[:, m * 128:(m + 1) * 128], lhsT=wqkv[:, k, off + m * 128:off + (m + 1) * 128],
                                           rhs=hnT[:, k, :], start=(k == 0), stop=(k == 7))
                    return ins
                P.add("pe", mmf, reads=["wqkv", hnT.name], writes=[pp.name])
                if nm == "QT":
                    P.add("act", lambda e, tt=tt, pp=pp, scale=scale: e.activation(out=tt[:].rearrange("p m t -> p (m t)"), in_=pp[:],
                                                                                  func=AF.Copy, scale=scale),
                          reads=[pp.name], writes=[tt.name])
                else:
                    P.add("dve", lambda e, tt=tt, pp=pp: e.tensor_copy(out=tt[:].rearrange("p m t -> p (m t)"), in_=pp[:]),
                          reads=[pp.name], writes=[tt.name])
                P.dma("sp", DR[:, :, j * 128:(j + 1) * 128], tt[:], reads=[tt.name], writes=[nm], partial=True)
            pv_, vt = pV[s], vts[s]

            def mmv(e, pv_=pv_, hnT=hnT):
                for k in range(8):
                    ins = e.matmul(pv_[:], lhsT=hnT[:, k, :], rhs=wqkv[:, k, 1024:1536], start=(k == 0), stop=(k == 7))
                return ins
            P.add("pe", mmv, reads=["wqkv", hnT.name], writes=[pv_.name])
            P.add("dve", lambda e, vt=vt, pv_=pv_: e.tensor_copy(out=vt[:], in_=pv_[:]), reads=[pv_.name], writes=[vt.name])
            P.dma("sp", VV[j * 128:(j + 1) * 128, :], vt[:], reads=[vt.name], writes=["VV"], partial=True)
        P.emit()

    def phase_b(L):
        P = Prog(nc)
        split = (L == 1)
        linit = LAMBDA_INIT[L]
        id32 = P.sbuf("id32B", [128, 128], F32)
        idb = P.sbuf("idbB", [128, 128], BF16)
        b32 = P.sbuf("b32B", [128, 4, 2, 128], F32)
        bia = P.sbuf("biaB", [128, 4, 2, 128], BF16)
        bfull = P.sbuf("bfullB", [128, 128], BF16)
        lam4 = P.sbuf("lam4", [128, 4, 64], F32)
        lamp = P.sbuf("lamp", [128, 2, 64], F32)
        lams = P.sbuf("lams", [128, 2], F32)
        nlam = P.sbuf("nlam", [128, 1], F32)
        sg_t = P.sbuf("sgB", [128, 128], F32)
        eps_t = P.sbuf("epsB", [128, 1], F32)
        ktb = [P.sbuf("ktB%d" % i, [128, TP], BF16) for i in range(2)]
        vtb = [P.sbuf("vtB%d" % i, [128, NBP, 132], BF16) for i in range(2)]
        qtb = [P.sbuf("qtB%d" % i, [128, 512], BF16) for i in range(2)]
        qab = [P.sbuf("qabB%d" % i, [128, 2, 512], BF16) for i in range(2)]
        cmb = P.sbuf("cmbB", [128, 4, 6, 128], BF16)
        pvt = P.sbuf("pvB", [128, 2], F32)
        ptb = [P.sbuf("ptB%d" % i, [128, 2, 512], BF16) for i in range(3)]
        pS = [P.psum("pSB%d" % i, [128, 2, 512], F32) for i in range(2)]
        pO = [P.psum("pOB%d" % i, [128, 2, 256], F32) for i in range(4)]
        osb = [P.sbuf("osbB%d" % i, [128, 2, 132], F32) for i in range(8)]
        r12 = [P.sbuf("r12B%d" % i, [128, 2], F32) for i in range(2)]
        ot = [P.sbuf("otB%d" % i, [128, 128], F32) for i in range(2)]
        junk = P.sbuf("junkB", [128, 128], F32)
        ss2 = [P.sbuf("ss2B%d" % i, [128, 1], F32) for i in range(2)]
        rs2 = [P.sbuf("rs2B%d" % i, [128, 1], F32) for i in range(2)]
        bo_t = [P.sbuf("boB%d" % i, [128, 128], BF16) for i in range(2)]

        P.dma("sp", id32[:], ident_in, writes=["id32B"])
        P.add("dve", lambda e: e.tensor_copy(out=idb[:], in_=id32[:]), reads=["id32B"], writes=["idbB"])
        P.dma("sp", b32[:], BT.rearrange("h (v k q) -> k h v q", v=2, k=128), writes=["b32B"])
        P.add("dve", lambda e: e.tensor_copy(out=bia[:], in_=b32[:]), reads=["b32B"], writes=["biaB"])
        P.add("pool", lambda e: e.memset(bfull[:], NEG), writes=["bfullB"])
        P.add("pool", lambda e: e.memset(eps_t[:], 1e-5), writes=["epsB"])
        for i, lv in enumerate((lq1, lk1, lq2, lk2)):
            P.dma("sp", lam4[:, i, :], lv[L:L + 1, :].broadcast_to([128, 64]), writes=["lam4"], partial=True)
        P.dma("sp", sg_t[:], subln[L:L + 1, :].broadcast_to([128, 128]), writes=["sgB"])
        if dbg and L == 0:
            P.dma("sp", DBG2[:, 0:256], lam4[:].rearrange("p a b -> p (a b)"), reads=["lam4"], writes=["DBG2"], partial=True)
        P.add("dve", lambda e: e.tensor_tensor(out=lamp[:, 0, :], in0=lam4[:, 0, :], in1=lam4[:, 1, :], op=ALU.mult),
              reads=["lam4"], writes=["lamp"])
        P.add("dve", lambda e: e.tensor_tensor(out=lamp[:, 1, :], in0=lam4[:, 2, :], in1=lam4[:, 3, :], op=ALU.mult),
              reads=["lam4", "lamp"], writes=["lamp"])
        for i in range(2):
            P.add("act", lambda e, i=i: e.activation(out=lam4[:, i, :], in_=lamp[:, i, :], func=AF.Copy, accum_out=lams[:, i:i + 1]),
                  reads=["lamp"], writes=["lams", "lam4"])
        P.add("act", lambda e: e.activation(out=lams[:], in_=lams[:], func=AF.Exp), reads=["lams"], writes=["lams"])
        P.add("dve", lambda e: e.tensor_tensor(out=nlam[:], in0=lams[:, 1:2], in1=lams[:, 0:1], op=ALU.subtract),
              reads=["lams"], writes=["nlam"])
        P.add("dve", lambda e: e.tensor_scalar(out=nlam[:], in0=nlam[:], scalar1=-linit, scalar2=0.0, op0=ALU.add, op1=ALU.add),
              reads=["nlam"], writes=["nlam"])
        P.add("dve", lambda e: e.tensor_scalar(out=sg_t[:], in0=sg_t[:], scalar1=(1.0 - linit), scalar2=0.0, op0=ALU.mult, op1=ALU.add),
              reads=["sgB"], writes=["sgB"])
        for i in range(2):
            P.add("pool", lambda e, i=i: e.memset(vtb[i][:], 1.0), writes=[vtb[i].name])
        if split:
            P.dma("sp", pvt[:], pv_in, writes=["pvB"])
            for hh in range(4):
                tabs = {"full": bfull[:], "prev": bia[:, hh, 1, :], "diag": bia[:, hh, 0, :]}
                for pi, (t0, t1) in enumerate((("full", None), ("full", "prev"), ("full", "diag"), ("full", "full"), ("diag", None), ("prev", None))):
                    P.add("dve", lambda e, hh=hh, pi=pi, a=tabs[t0]: e.tensor_scalar(out=cmb[:, hh, pi, :], in0=a, scalar1=pvt[:, 1:2], scalar2=0.0,
                                                                                   op0=ALU.mult, op1=ALU.add),
                          reads=["biaB", "bfullB", "pvB"], writes=["cmbB"])
                    if t1 is not None:
                        P.add("dve", lambda e, hh=hh, pi=pi, b=tabs[t1]: e.scalar_tensor_tensor(out=cmb[:, hh, pi, :], in0=b, scalar=pvt[:, 0:1],
                                                                                              in1=cmb[:, hh, pi, :], op0=ALU.mult, op1=ALU.add),
                              reads=["biaB", "bfullB", "pvB", "cmbB"], writes=["cmbB"])
        VVv = VV.rearrange("(n p) (h d) -> p n h d", p=128, h=4)
        ngroups = (NB + 3) // 4
        sidx = 0
        qidx = 0
        eidx = 0
        def head_loads(h):
            kt, vt = ktb[h % 2], vtb[h % 2]
            P.dma("sp", kt[:], KT[h], writes=[kt.name])
            for n0 in range(0, NBP, 8):
                n1 = min(NBP, n0 + 8)
                P.dma("sp", vt[:, n0:n1, 0:128], VVv[:, n0:n1, h, :], writes=[vt.name], partial=True)

        for qi in range(4):
            P.add("dve", lambda e, qi=qi: e.memset(pO[qi][:], 0.0), writes=["pO%d_0" % qi, "pO%d_1" % qi])
        head_loads(0)
        pend_epi = [None]
        oidx = 0
        eidx_box = [0]
        for h in range(4):
            kt, vt = ktb[h % 2], vtb[h % 2]
            if h + 1 < 4:
                head_loads(h + 1)
            for g in range(NI if split else ngroups):
                qt = qtb[qidx % 2]
                if split:
                    qb0 = 8 * g
                    nq = 4
                    W = 512
                    qa = qab[qidx % 2]
                    P.dma("sp", qa[:, 0, :], QT[h][:, qb0 * 128:qb0 * 128 + 512], writes=[qa.name], partial=True)
                    P.dma("sp", qa[:, 1, :], QT[h][:, (qb0 + 4) * 128:(qb0 + 4) * 128 + 512], writes=[qa.name], partial=True)
                    P.add("dve", lambda e, qt=qt, qa=qa: e.tensor_scalar(out=qt[:], in0=qa[:, 0, :], scalar1=pvt[:, 1:2], scalar2=0.0,
                                                                       op0=ALU.mult, op1=ALU.add),
                          reads=[qa.name, "pvB"], writes=[qt.name])
                    P.add("dve", lambda e, qt=qt, qa=qa: e.scalar_tensor_tensor(out=qt[:], in0=qa[:, 1, :], scalar=pvt[:, 0:1], in1=qt[:],
                                                                              op0=ALU.mult, op1=ALU.add),
                          reads=[qa.name, "pvB", qt.name], writes=[qt.name])
                    last_kb = qb0 + 7
                else:
                    qb0 = g * 4
                    nq = min(4, NB - qb0)
                    W = nq * 128
                    P.dma("sp", qt[:, 0:W], QT[h][:, qb0 * 128:qb0 * 128 + W], writes=[qt.name])
                    last_kb = qb0 + nq - 1
                qidx += 1

                def near_tab(kb, qi, qb0=qb0, h=h):
                    rel = qb0 + qi - kb
                    if not split:
                        if rel >= 2:
                            return None
                        return bia[:, h, 1, :] if rel == 1 else (bia[:, h, 0, :] if rel == 0 else bfull[:])
                    f = lambda r: None if r >= 2 else ("prev" if r == 1 else ("diag" if r == 0 else "full"))
                    pr = (f(rel), f(rel + 4))
                    if pr == (None, None):
                        return None
                    pi = {("full", None): 0, ("full", "prev"): 1, ("full", "diag"): 2, ("full", "full"): 3, ("diag", None): 4, ("prev", None): 5}[pr]
                    return cmb[:, h, pi, :]

                def pv_last(qi, qb0=qb0, last_kb=last_kb):
                    return min(last_kb, qb0 + qi + (4 if split else 0))
                def emit_s(kb, slS, slP, kt=kt, qt=qt, W=W, qb0=qb0, nq=nq, h=h, near_tab=near_tab):
                    S2 = pS[slS]
                    pt2 = ptb[slP]

                    q0 = max(0, kb - (qb0 + (4 if split else 0)))
                    C0 = q0 * 128

                    def smm(e, kb=kb):
                        for c in range(2):
                            extra = []
                            if kb >= qb0 - 1:
                                for qi in range(q0, nq):
                                    tb = near_tab(kb, qi)
                                    if tb is not None:
                                        extra.append((qi, tb))
                            ins = e.matmul(S2[:, c, C0:W], lhsT=kt[c * 64:(c + 1) * 64, kb * 128:(kb + 1) * 128],
                                           rhs=qt[c * 64:(c + 1) * 64, C0:W], start=True, stop=(len(extra) == 0))
                            for n_, (qi, bap) in enumerate(extra):
                                ins = e.matmul(S2[:, c, qi * 128:(qi + 1) * 128], lhsT=idb[:], rhs=bap, start=False,
                                               stop=(n_ == len(extra) - 1))
                        return ins
                    P.add("pe", smm, reads=[kt.name, qt.name, "biaB", "bfullB", "idbB", "cmbB"], writes=[S2.name])
                    P.add("act", lambda e: e.activation(out=pt2[:, :, C0:W], in_=S2[:, :, C0:W], func=AF.Exp),
                          reads=[S2.name], writes=[pt2.name])

                def emit_pv(kb, slP, vt=vt, qb0=qb0, nq=nq, pv_last=pv_last):
                    pt2 = ptb[slP]

                    def pvmm(e, kb=kb):
                        ins = None
                        for c in range(2):
                            for qi in range(nq):
                                if kb > pv_last(qi):
                                    continue
                                ins = e.matmul(pO[qi][:, c, 0:129], lhsT=pt2[:, c, qi * 128:(qi + 1) * 128], rhs=vt[:, kb, 0:129],
                                               start=False, stop=(kb == pv_last(qi)), skip_group_check=True)
                        return ins
                    P.add("pe", pvmm, reads=[pt2.name, vt.name],
                          writes=["pO%d_%d" % (qi, c) for c in range(2) for qi in range(nq) if kb <= pv_last(qi)])

                pendq = []
                for kb in range(last_kb + 1):
                    slS = sidx % 2
                    slP = sidx % 3
                    sidx += 1
                    emit_s(kb, slS, slP)
                    pendq.append((kb, slP))
                    if len(pendq) > 2:
                        emit_pv(*pendq.pop(0))
                    if kb == 3 and pend_epi[0] is not None:
                        pend_epi[0]()
                        pend_epi[0] = None
                while pendq:
                    emit_pv(*pendq.pop(0))
                if pend_epi[0] is not None:
                    pend_epi[0]()
                    pend_epi[0] = None
                oslots = []
                for qi in range(nq):
                    ob = osb[oidx % 8]
                    oidx += 1
                    oslots.append(ob)
                    rn = ["pO%d_0" % qi, "pO%d_1" % qi]
                    P.add("dve", lambda e, ob=ob, qi=qi: e.tensor_copy(out=ob[:, :, 0:129], in_=pO[qi][:, :, 0:129]), reads=rn, writes=[ob.name])
                    P.add("dve", lambda e, qi=qi: e.memset(pO[qi][:], 0.0), writes=rn)

                def epi(oslots=oslots, qb0=qb0, nq=nq, h=h):
                    nonlocal_e = eidx_box
                    for qi in range(nq):
                        qb = qb0 + qi
                        es = nonlocal_e[0] % 2
                        nonlocal_e[0] += 1
                        r, o, s2, rr, bo = r12[es], ot[es], ss2[es], rs2[es], bo_t[es]
                        ob = oslots[qi]
                        P.add("dve", lambda e, r=r, ob=ob: e.reciprocal(out=r[:].rearrange("p (c o) -> p c o", o=1), in_=ob[:, :, 128:129]),
                              reads=[ob.name], writes=[r.name])
                        P.add("dve", lambda e, r=r: e.tensor_tensor(out=r[:, 1:2], in0=r[:, 1:2], in1=nlam[:], op=ALU.mult),
                              reads=[r.name, "nlam"], writes=[r.name])
                        P.add("dve", lambda e, r=r, ob=ob, o=o: e.tensor_scalar(out=o[:], in0=ob[:, 0, 0:128], scalar1=r[:, 0:1], scalar2=0.0,
                                                                              op0=ALU.mult, op1=ALU.add),
                              reads=[ob.name, r.name], writes=[o.name])
                        P.add("dve", lambda e, r=r, ob=ob, o=o: e.scalar_tensor_tensor(out=o[:], in0=ob[:, 1, 0:128], scalar=r[:, 1:2], in1=o[:],
                                                                                     op0=ALU.mult, op1=ALU.add),
                              reads=[ob.name, r.name, o.name], writes=[o.name])
                        P.add("dve", lambda e, o=o, s2=s2: e.scalar_tensor_tensor(out=junk[:], in0=o[:], scalar=1.0, in1=o[:], op0=ALU.mult, op1=ALU.mult,
                                                                                accum_out=s2[:]),
                              reads=[o.name], writes=["junkB", s2.name])
                        P.add("act", lambda e, s2=s2, rr=rr: e.activation(out=rr[:], in_=s2[:], func=AF.Ln, bias=eps_t[:], scale=1.0 / 128),
                              reads=[s2.name, "epsB"], writes=[rr.name])
                        P.add("act", lambda e, rr=rr: e.activation(out=rr[:], in_=rr[:], func=AF.Exp, scale=-0.5), reads=[rr.name], writes=[rr.name])
                        P.add("dve", lambda e, o=o, rr=rr, bo=bo: e.scalar_tensor_tensor(out=bo[:], in0=o[:], scalar=rr[:, 0:1], in1=sg_t[:],
                                                                                       op0=ALU.mult, op1=ALU.mult),
                              reads=[o.name, rr.name, "sgB"], writes=[bo.name])
                        if split:
                            sl_ = (qb0 // 8) * 4 + qi
                            P.dma("sp", BO1[sl_ * 128:(sl_ + 1) * 128, h * 128:(h + 1) * 128], bo[:], reads=[bo.name], writes=["BO"], partial=True)
                        else:
                            P.dma("sp", BO[qb * 128:(qb + 1) * 128, h * 128:(h + 1) * 128], bo[:], reads=[bo.name], writes=["BO"], partial=True)
                pend_epi[0] = epi
        if pend_epi[0] is not None:
            pend_epi[0]()
            pend_epi[0] = None
        P.emit()

    def phase_c(L):
        P = Prog(nc)
        split = (L == 1)
        NBLK = NS if split else NB
        src = xin if L == 0 else H1
        stg = [P.sbuf("stgC%d" % i, [128, 2048], F32) for i in range(2)]
        wu_ = P.sbuf("wuC", [128, 8, 512], BF16)
        wgt = P.sbuf("wgtC", [128, 8, 2048], BF16)
        wpu = P.sbuf("wpuC", [128, 4, D], BF16)
        wau = P.sbuf("wauC", [128, 4, D], BF16)
        wo = P.sbuf("woC", [128, 8, D], BF16)
        gw = P.sbuf("gwC", [128, 4, 128], BF16)
        psc = P.sbuf("pscC", [128, 4], F32)
        inv = P.sbuf("invC", [128, 2, 4, 128], F32)
        id32 = P.sbuf("id32C", [128, 128], F32)
        idb = P.sbuf("idbC", [128, 128], BF16)
        xab = [P.sbuf("xabC%d" % i, [128, 2, 8, 144], BF16) for i in range(2)]
        pvt = P.sbuf("pvC", [128, 2], F32)
        inv0 = P.sbuf("inv0C", [128, 4, 128], F32)
        hx = [P.sbuf("hxC%d" % i, [128, 8, 144], BF16) for i in range(2)]
        hts = [P.sbuf("hC%d" % i, [128, D], F32) for i in range(3)]
        bos = [P.sbuf("boC%d" % i, [128, 512], BF16) for i in range(2)]
        u32s = [P.sbuf("u32C%d" % i, [128, 4, 144], F32) for i in range(2)]
        s2 = P.sbuf("s2C", [128, 4, 144], F32)
        s4 = P.sbuf("s4C", [128, 4, 144], F32)
        s8 = P.sbuf("s8C", [128, 4, 144], F32)
        s16 = P.sbuf("s16C", [128, 4, 144], F32)
        avg = P.sbuf("avgC", [128, 4, 128], F32)
        mixs = [P.sbuf("mixC%d" % i, [128, 4, 128], BF16) for i in range(2)]
        aTs = [P.sbuf("aTC%d" % i, [128, 4, 128], BF16) for i in range(1)] * 2
        boTs = [P.sbuf("boTC%d" % i, [128, 4, 128], BF16) for i in range(2)]
        sig0s = [P.sbuf("sig0C%d" % i, [128, D], F32) for i in range(2)]
        sig1s = [P.sbuf("sig1C%d" % i, [128, D], F32) for i in range(2)]
        m0 = P.sbuf("m0C", [128, D], F32)
        m1s = [P.sbuf("m1C%d" % i, [128, D], F32) for i in range(1)] * 2
        mgs = [P.sbuf("mgC%d" % i, [128, D], BF16) for i in range(1)] * 2
        mgTs = [P.sbuf("mgTC%d" % i, [128, 8, 128], BF16) for i in range(1)] * 2
        hms = [P.sbuf("hmC%d" % i, [128, D], F32) for i in range(2)]
        pU = [P.psum("pUC%d" % i, [128, 512], F32) for i in range(2)]
        pY = P.psum("pYC", [128, 512], F32)
        pG = P.psum("pGC", [128, 1024], F32)
        pX = P.psum("pXC", [128, 1024], F32)
        pTr = P.psum("pTrC", [128, 512], F32)

        P.dma("sp", id32[:], ident_in, writes=["id32C"])
        P.add("dve", lambda e: e.tensor_copy(out=idb[:], in_=id32[:]), reads=["id32C"], writes=["idbC"])
        for g in range(4):
            P.dma("sp", psc[:, g:g + 1], pool_scale[L, g * 128:(g + 1) * 128].rearrange("(p o) -> p o", o=1), writes=["pscC"], partial=True)
        P.dma("sp", inv[:].rearrange("p a g t -> p (a g t)"), invc_in.broadcast_to([128, 1024]), writes=["invC"])
        P.dma("sp", pvt[:], pv_in, writes=["pvC"])
        P.add("dve", lambda e: e.tensor_scalar(out=inv0[:], in0=inv[:, 1, :, :], scalar1=pvt[:, 1:2], scalar2=0.0, op0=ALU.mult, op1=ALU.add),
              reads=["invC", "pvC"], writes=["inv0C"])
        P.add("dve", lambda e: e.scalar_tensor_tensor(out=inv0[:], in0=inv[:, 0, :, :], scalar=pvt[:, 0:1], in1=inv0[:], op0=ALU.mult, op1=ALU.add),
              reads=["invC", "pvC", "inv0C"], writes=["inv0C"])
        load_cast(P, wu_, w_in[L][:, 0:512], 512, stg, "wuC")
        load_cast(P, wgt, w_in[L][:, 2048:4096], 2048, stg, "wgtC")
        load_cast(P, wpu, w_pu[L], D, stg, "wpuC", nchunk=4)
        load_cast(P, wau, w_au[L], D, stg, "wauC", nchunk=4)
        load_cast(P, wo, w_out[L], D, stg, "woC")
        sv = stg[0][:, 0:512].rearrange("p (g d) -> p g d", d=128)
        P.dma("sp", sv, pool_gw[L].rearrange("g c d -> c g d"), writes=[stg[0].name])
        P.add("dve", lambda e: e.tensor_copy(out=gw[:], in_=sv), reads=[stg[0].name], writes=["gwC"])
        def issue_loads(j):
            s = j % 2
            x_, h, bo = hx[s], hts[j % 3], bos[s]
            if not split:
                if j == 0:
                    P.add("pool", lambda e, x_=x_: e.memset(x_[:, :, 0:16], 0.0), writes=[x_.name])
                    P.dma("sp", x_[:, :, 16:144], HNT[:, :, 0:128], writes=[x_.name])
                else:
                    P.dma("sp", x_[:], HNT[:, :, j * 128 - 16:(j + 1) * 128], writes=[x_.name])
                P.dma("sp", h[:], src[j * 128:(j + 1) * 128, :], writes=[h.name])
                P.dma("sp", bo[:], BO[j * 128:(j + 1) * 128, :], writes=[bo.name])
                return
            i_, qi_ = j // 4, j % 4
            jA = 8 * i_ + qi_
            jB = jA + 4
            xa = xab[s]
            hs = stg[s]
            if jA == 0:
                P.add("pool", lambda e, xa=xa: e.memset(xa[:, 0, :, 0:16], 0.0), writes=[xa.name])
                P.dma("sp", xa[:, 0, :, 16:144], HNT[:, :, 0:128], writes=[xa.name])
            else:
                P.dma("sp", xa[:, 0, :, :], HNT[:, :, jA * 128 - 16:(jA + 1) * 128], writes=[xa.name], partial=True)
            P.dma("sp", xa[:, 1, :, :], HNT[:, :, jB * 128 - 16:(jB + 1) * 128], writes=[xa.name], partial=True)
            P.dma("sp", hs[:, 0:1024], src[jA * 128:(jA + 1) * 128, :], writes=[hs.name], partial=True)
            P.dma("sp", hs[:, 1024:2048], src[jB * 128:(jB + 1) * 128, :], writes=[hs.name], partial=True)
            P.dma("sp", bo[:], BO1[j * 128:(j + 1) * 128, :], writes=[bo.name])
            P.add("pool", lambda e, x_=x_, xa=xa: e.tensor_scalar(out=x_[:], in0=xa[:, 0, :, :], scalar1=pvt[:, 1:2], scalar2=0.0, op0=ALU.mult, op1=ALU.add),
                  reads=[xa.name, "pvC"], writes=[x_.name])
            P.add("dve", lambda e, x_=x_, xa=xa: e.scalar_tensor_tensor(out=x_[:], in0=xa[:, 1, :, :], scalar=pvt[:, 0:1], in1=x_[:], op0=ALU.mult, op1=ALU.add),
                  reads=[xa.name, "pvC", x_.name], writes=[x_.name])
            P.add("pool", lambda e, h=h, hs=hs: e.tensor_scalar(out=h[:], in0=hs[:, 0:1024], scalar1=pvt[:, 1:2], scalar2=0.0, op0=ALU.mult, op1=ALU.add),
                  reads=[hs.name, "pvC"], writes=[h.name])
            P.add("dve", lambda e, h=h, hs=hs: e.scalar_tensor_tensor(out=h[:], in0=hs[:, 1024:2048], scalar=pvt[:, 0:1], in1=h[:], op0=ALU.mult, op1=ALU.add),
                  reads=[hs.name, "pvC", h.name], writes=[h.name])

        ptr_b = pTr[:].bitcast(BF16)

        def slot(j):
            s = j % 2
            return dict(x_=hx[s], h=hts[j % 3], bo=bos[s], hm=hms[s], u32=u32s[s], mix=mixs[s], aT=aTs[s], boT=boTs[s], sig0=sig0s[s],
                        sig1=sig1s[s], m1=m1s[s], mg=mgs[s], mgT=mgTs[s])

        def gmm(e, x_, off):
            for n in range(2):
                for k in range(8):
                    ins = e.matmul(pG[:, n * 512:(n + 1) * 512], lhsT=x_[:, k, 16:144], rhs=wgt[:, k, off + n * 512:off + (n + 1) * 512],
                                   start=(k == 0), stop=(k == 7))
            return ins

        def e_umm(j):
            t = slot(j)
            x_, u32 = t["x_"], t["u32"]
            for half in range(2):
                pu = pU[half]

                def umm(e, pu=pu, half=half):
                    for gg in range(2):
                        g = half * 2 + gg
                        for k in range(8):
                            ins = e.matmul(pu[:, gg * 144:(gg + 1) * 144], lhsT=wu_[:, k, g * 128:(g + 1) * 128], rhs=x_[:, k, :],
                                           start=(k == 0), stop=(k == 7))
                    return ins
                P.add("pe", umm, reads=["wuC", x_.name], writes=[pu.name])
                P.add("act", lambda e, pu=pu, half=half: e.activation(out=u32[:, half * 2:half * 2 + 2, :].rearrange("p g t -> p (g t)"),
                                                                    in_=pu[:, 0:288], func=AF.Copy),
                      reads=[pu.name], writes=[u32.name])

        def e_gate(j, which):
            t = slot(j)
            x_ = t["x_"]
            sg = t["sig0"] if which == 0 else t["sig1"]
            P.add("pe", lambda e: gmm(e, x_, which * 1024), reads=["wgtC", x_.name], writes=["pGC"])
            P.add("act", lambda e: e.activation(out=sg[:], in_=pG[:], func=AF.Sigmoid), reads=["pGC"], writes=[sg.name])

        def e_trb(j):
            t = slot(j)
            bo, boT = t["bo"], t["boT"]

            def trb(e):
                for c in range(4):
                    ins = e.transpose(out=ptr_b[:, c * 128:(c + 1) * 128], in_=bo[:, c * 128:(c + 1) * 128], identity=idb[:])
                return ins
            P.add("pe", trb, reads=[bo.name, "idbC"], writes=["pTrC"])
            P.add("dve", lambda e: e.tensor_copy(out=boT[:].rearrange("p c t -> p (c t)"), in_=ptr_b[:, 0:512]), reads=["pTrC"], writes=[boT.name])

        def e_pool(j):
            t = slot(j)
            u32, mix = t["u32"], t["mix"]
            P.add("dve", lambda e: e.tensor_tensor(out=s2[:, :, 1:144], in0=u32[:, :, 1:144], in1=u32[:, :, 0:143], op=ALU.add),
                  reads=[u32.name], writes=["s2C"])
            P.add("dve", lambda e: e.tensor_tensor(out=s4[:, 1:4, 3:144], in0=s2[:, 1:4, 3:144], in1=s2[:, 1:4, 1:142], op=ALU.add),
                  reads=["s2C"], writes=["s4C"])
            P.add("dve", lambda e: e.tensor_tensor(out=s8[:, 2:4, 7:144], in0=s4[:, 2:4, 7:144], in1=s4[:, 2:4, 3:140], op=ALU.add),
                  reads=["s4C"], writes=["s8C"])
            P.add("dve", lambda e: e.tensor_tensor(out=s16[:, 3:4, 15:144], in0=s8[:, 3:4, 15:144], in1=s8[:, 3:4, 7:136], op=ALU.add),
                  reads=["s8C"], writes=["s16C"])
            for g, (st, sn) in enumerate(((s2, "s2C"), (s4, "s4C"), (s8, "s8C"), (s16, "s16C"))):
                if j == 0:
                    ivap = inv0[:, g, :] if split else inv[:, 1, g, :]
                else:
                    ivap = inv[:, 0, g, :]
                P.add("dve", lambda e, g=g, st=st, ivap=ivap: e.tensor_tensor(out=avg[:, g, :], in0=st[:, g, 16:144], in1=ivap, op=ALU.mult),
                      reads=[sn, "invC", "inv0C"], writes=["avgC"])
            P.add("dve", lambda e: e.tensor_tensor(out=mix[:], in0=avg[:], in1=u32[:, :, 16:144], op=ALU.subtract),
                  reads=["avgC", u32.name], writes=[mix.name])

        def l_ymm(j):
            t = slot(j)
            mix, aT = t["mix"], t["aT"]

            def ymm(e):
                for g in range(4):
                    ins = e.matmul(pY[:, g * 128:(g + 1) * 128], lhsT=gw[:, g, :], rhs=mix[:, g, :], start=True, stop=True)
                return ins
            P.add("pe", ymm, reads=["gwC", mix.name], writes=["pYC"])
            for g in range(4):
                P.add("dve", lambda e, g=g: e.tensor_scalar(out=aT[:, g, :], in0=pY[:, g * 128:(g + 1) * 128], scalar1=psc[:, g:g + 1], scalar2=0.0,
                                                            op0=ALU.mult, op1=ALU.add),
                      reads=["pYC", "pscC"], writes=[aT.name])

        def l_aup(j):
            t = slot(j)
            aT, sig0 = t["aT"], t["sig0"]

            def aup(e):
                for n in range(2):
                    for g in range(4):
                        ins = e.matmul(pX[:, n * 512:(n + 1) * 512], lhsT=aT[:, g, :], rhs=wpu[:, g, n * 512:(n + 1) * 512],
                                       start=(g == 0), stop=(g == 3))
                return ins
            P.add("pe", aup, reads=["wpuC", aT.name], writes=["pXC"])
            P.add("dve", lambda e: e.tensor_tensor(out=m0[:], in0=sig0[:], in1=pX[:], op=ALU.mult), reads=[sig0.name, "pXC"], writes=["m0C"])

        def l_bup(j):
            t = slot(j)
            boT, sig1, m1, mg = t["boT"], t["sig1"], t["m1"], t["mg"]

            def bup(e):
                for n in range(2):
                    for g in range(4):
                        ins = e.matmul(pX[:, n * 512:(n + 1) * 512], lhsT=boT[:, g, :], rhs=wau[:, g, n * 512:(n + 1) * 512],
                                       start=(g == 0), stop=(g == 3))
                return ins
            P.add("pe", bup, reads=["wauC", boT.name], writes=["pXC"])
            P.add("dve", lambda e: e.tensor_tensor(out=m1[:], in0=sig1[:], in1=pX[:], op=ALU.mult), reads=[sig1.name, "pXC"], writes=[m1.name])
            P.add("pool", lambda e: e.tensor_tensor(out=mg[:], in0=m0[:], in1=m1[:], op=ALU.add), reads=["m0C", m1.name], writes=[mg.name])

        def l_trm(j):
            t = slot(j)
            mg, mgT = t["mg"], t["mgT"]

            def trm(e):
                for c in range(8):
                    ins = e.transpose(out=ptr_b[:, c * 128:(c + 1) * 128], in_=mg[:, c * 128:(c + 1) * 128], identity=idb[:])
                return ins
            P.add("pe", trm, reads=[mg.name, "idbC"], writes=["pTrC"])
            P.add("act", lambda e: e.activation(out=mgT[:].rearrange("p c t -> p (c t)"), in_=ptr_b, func=AF.Copy),
                  reads=["pTrC"], writes=[mgT.name])

        def l_omm(j):
            t = slot(j)
            mgT, h, hm = t["mgT"], t["h"], t["hm"]

            def omm(e):
                for n in range(2):
                    for k in range(8):
                        ins = e.matmul(pX[:, n * 512:(n + 1) * 512], lhsT=mgT[:, k, :], rhs=wo[:, k, n * 512:(n + 1) * 512],
                                       start=(k == 0), stop=(k == 7))
                return ins
            P.add("pe", omm, reads=["woC", mgT.name], writes=["pXC"])
            P.add("dve", lambda e: e.tensor_tensor(out=hm[:], in0=pX[:], in1=h[:], op=ALU.add), reads=["pXC", h.name], writes=[hm.name])
            P.dma("sp", (HMO if split else HM)[j * 128:(j + 1) * 128, :], hm[:], reads=[hm.name], writes=["HM"], partial=True)

        issue_loads(0)
        for j in range(NBLK + 1):
            je, jl = (j if j < NBLK else None), (j - 1 if j >= 1 else None)
            if je is not None and je + 1 < NBLK:
                issue_loads(je + 1)
            if je is not None:
                e_umm(je)
            if jl is not None:
                l_ymm(jl)
            if je is not None:
                e_gate(je, 0)
            if jl is not None:
                l_aup(jl)
            if je is not None:
                e_trb(je)
            if jl is not None:
                l_bup(jl)
            if je is not None:
                e_pool(je)
                e_gate(je, 1)
            if jl is not None:
                l_trm(jl)
                l_omm(jl)
        P.emit()

    def phase_d(L):
        moe = (L == 1)
        nblk = NBH if moe else NB
        GMAX = 13
        ngr = (nblk + GMAX - 1) // GMAX
        base = nblk // ngr
        rem = nblk % ngr
        gsz = [base + (1 if i < rem else 0) for i in range(ngr)]
        GM = max(gsz)
        nexp = NEXP if moe else 1
        FC = 256
        nfc = DFF // FC
        blk0 = 0
        for gi, G in enumerate(gsz):
            P = Prog(nc)
            acc = P.sbuf("accD", [128, GM, D], F32)
            hnT = P.sbuf("hnTD", [128, 8, GM * 128], BF16)
            g_t = P.sbuf("gD", [128, D], F32)
            eps_t = P.sbuf("epsD", [128, 1], F32)
            id32 = P.sbuf("id32D", [128, 128], F32)
            idb = P.sbuf("idbD", [128, 128], BF16)
            pvt = P.sbuf("pvD", [128, 2], F32)
            hb = [P.sbuf("hbD%d" % i, [128, D], F32) for i in range(2)]
            junk = P.sbuf("junkD", [128, D], BF16)
            sss = [P.sbuf("ssD%d" % i, [128, 1], F32) for i in range(2)]
            rss = [P.sbuf("rsD%d" % i, [128, 1], F32) for i in range(2)]
            hns = [P.sbuf("hnD%d" % i, [128, D], BF16) for i in range(2)]
            sgw = [P.sbuf("sgwD%d" % i, [128, 8, FC], F32) for i in range(2)]
            suw = [P.sbuf("suwD%d" % i, [128, 8, FC], F32) for i in range(2)]
            sdw = [P.sbuf("sdwD%d" % i, [128, 2, D], F32) for i in range(2)]
            wgb = [P.sbuf("wgbD%d" % i, [128, 8, FC], BF16) for i in range(2)]
            wub = [P.sbuf("wubD%d" % i, [128, 8, FC], BF16) for i in range(2)]
            wdb = [P.sbuf("wdbD%d" % i, [128, 2, D], BF16) for i in range(2)]
            sgt = [P.sbuf("sgtD%d" % i, [128, 256], F32) for i in range(4)]
            actT = [P.sbuf("actD%d" % i, [128, 2, 256], BF16) for i in range(2)]
            pGU = [P.psum("pGUD%d" % i, [128, 2, 256], F32) for i in range(4)]
            pDN = [P.psum("pDND%d" % i, [128, 1024], F32) for i in range(2)]
            pTr_t, pR_t = pGU[2], pGU[3]
            pTr = pTr_t[:].rearrange("p a b -> p (a b)")
            pR = pR_t[:].rearrange("p a b -> p (a b)")
            if moe:
                rt32 = P.sbuf("rt32D", [128, 8, NEXP], F32)
                rtb = P.sbuf("rtbD", [128, 8, NEXP], BF16)
                cw = P.sbuf("cwD", [128, GM, NEXP], F32)
                lg = P.sbuf("lgD", [128, NEXP], F32)
                lg2 = P.sbuf("lg2D", [128, NEXP], F32)
                eq = P.sbuf("eqD", [128, NEXP], F32)
                ex = P.sbuf("exD", [128, NEXP], F32)
                mm1 = P.sbuf("mm1D", [128, 1], F32)
                mm2 = P.sbuf("mm2D", [128, 1], F32)
                sm = P.sbuf("smD", [128, 1], F32)
            if L == 1:
                gf = P.sbuf("gfD", [128, D], F32)
                P.dma("sp", gf[:], final_norm.broadcast_to([128, D]), writes=["gfD"])
            P.dma("sp", g_t[:], norm_ffn[L:L + 1, :].broadcast_to([128, D]), writes=["gD"])
            P.add("pool", lambda e: e.memset(eps_t[:], 1e-6), writes=["epsD"])
            P.dma("sp", id32[:], ident_in, writes=["id32D"])
            P.add("dve", lambda e: e.tensor_copy(out=idb[:], in_=id32[:]), reads=["id32D"], writes=["idbD"])
            P.dma("sp", pvt[:], pv_in, writes=["pvD"])
            if moe:
                P.dma("sp", rt32[:], router[0].rearrange("(c p) n -> p c n", p=128), writes=["rt32D"])
                P.add("dve", lambda e: e.tensor_copy(out=rtb[:], in_=rt32[:]), reads=["rt32D"], writes=["rtbD"])
            for i in range(G):
                jb = blk0 + i
                s = i % 2
                a_ap = acc[:, i, :]
                ares = "acc%d" % i
                if moe:
                    i_, qi_ = SL[jb]
                    sl_ = 4 * i_ + qi_
                    P.dma("sp", a_ap, HMO[sl_ * 128:(sl_ + 1) * 128, :], writes=[ares])
                else:
                    P.dma("sp", a_ap, HM[jb * 128:(jb + 1) * 128, :], writes=[ares])
                ss, rs, hn = sss[s], rss[s], hns[s]
                rmsnorm_block(P, a_ap, ares, g_t, eps_t, junk, ss, rs, hn, hn.name)
                ptr_b = pTr[:].bitcast(BF16)

                def tr(e, hn=hn, ptr_b=ptr_b):
                    for c in range(8):
                        ins = e.transpose(out=ptr_b[:, c * 128:(c + 1) * 128], in_=hn[:, c * 128:(c + 1) * 128], identity=idb[:])
                    return ins
                P.add("pe", tr, reads=[hn.name, "idbD"], writes=[pTr_t.name])
                P.add("act", lambda e, i=i, ptr_b=ptr_b: e.activation(out=hnT[:, :, i * 128:(i + 1) * 128],
                                                                    in_=ptr_b.rearrange("p (c t) -> p c t", t=128), func=AF.Copy),
                      reads=[pTr_t.name], writes=["hnT%d" % i])
                if moe:
                    def rmm(e, i=i):
                        for k in range(8):
                            ins = e.matmul(pR[:, 0:NEXP], lhsT=hnT[:, k, i * 128:(i + 1) * 128], rhs=rtb[:, k, :], start=(k == 0), stop=(k == 7))
                        return ins
                    P.add("pe", rmm, reads=["rtbD", "hnT%d" % i], writes=[pR_t.name])
                    P.add("dve", lambda e: e.tensor_copy(out=lg[:], in_=pR[:, 0:NEXP]), reads=[pR_t.name], writes=["lgD"])
                    P.add("dve", lambda e: e.reduce_max(out=mm1[:], in_=lg[:], axis=AX.X), reads=["lgD"], writes=["mm1D"])
                    P.add("dve", lambda e: e.tensor_scalar(out=eq[:], in0=lg[:], scalar1=mm1[:, 0:1], scalar2=0.0, op0=ALU.is_equal, op1=ALU.add),
                          reads=["lgD", "mm1D"], writes=["eqD"])
                    P.add("dve", lambda e: e.scalar_tensor_tensor(out=lg2[:], in0=eq[:], scalar=-1e30, in1=lg[:], op0=ALU.mult, op1=ALU.add),
                          reads=["eqD", "lgD"], writes=["lg2D"])
                    P.add("dve", lambda e: e.reduce_max(out=mm2[:], in_=lg2[:], axis=AX.X), reads=["lg2D"], writes=["mm2D"])
                    P.add("dve", lambda e: e.tensor_scalar(out=eq[:], in0=lg[:], scalar1=mm2[:, 0:1], scalar2=0.0, op0=ALU.is_ge, op1=ALU.add),
                          reads=["lgD", "mm2D", "eqD"], writes=["eqD"])
                    P.add("dve", lambda e: e.tensor_scalar(out=mm1[:], in0=mm1[:], scalar1=-1.0, scalar2=0.0, op0=ALU.mult, op1=ALU.add),
                          reads=["mm1D"], writes=["mm1D"])
                    P.add("act", lambda e: e.activation(out=ex[:], in_=lg[:], func=AF.Exp, bias=mm1[:], scale=1.0),
                          reads=["lgD", "mm1D"], writes=["exD"])
                    P.add("dve", lambda e: e.tensor_tensor(out=ex[:], in0=ex[:], in1=eq[:], op=ALU.mult), reads=["exD", "eqD"], writes=["exD"])
                    P.add("dve", lambda e: e.reduce_sum(out=sm[:], in_=ex[:], axis=AX.X), reads=["exD"], writes=["smD"])
                    P.add("dve", lambda e: e.reciprocal(out=sm[:], in_=sm[:]), reads=["smD"], writes=["smD"])
                    P.add("dve", lambda e, i=i: e.tensor_scalar(out=cw[:, i, :], in0=ex[:], scalar1=sm[:, 0:1], scalar2=0.0, op0=ALU.mult, op1=ALU.add),
                          reads=["exD", "smD"], writes=["cw%d" % i])
            tiles = [(t0, min(2, G - t0)) for t0 in range(0, G, 2)]
            wi = 0
            gi_ = 0
            ti_ = 0
            pend = [None]
            for ex_i in range(nexp):
                if moe:
                    Wg, Wu, Wd = mwg[0][ex_i], mwu[0][ex_i], mwd[0][ex_i]
                else:
                    Wg, Wu, Wd = dwg[0], dwu[0], dwd[0]
                Wgv = Wg.rearrange("(c p) n -> p c n", p=128)
                Wuv = Wu.rearrange("(c p) n -> p c n", p=128)
                Wdv = Wd.rearrange("(c p) n -> p c n", p=128)
                for fc in range(nfc):
                    ws = wi % 2
                    wi += 1
                    f0 = fc * FC
                    P.dma("sp", sgw[ws][:], Wgv[:, :, f0:f0 + FC], writes=[sgw[ws].name])
                    P.dma("sp", suw[ws][:], Wuv[:, :, f0:f0 + FC], writes=[suw[ws].name])
                    P.dma("sp", sdw[ws][:], Wdv[:, fc * 2:fc * 2 + 2, :], writes=[sdw[ws].name])
                    P.add("pool", lambda e, ws=ws: e.tensor_copy(out=wgb[ws][:], in_=sgw[ws][:]), reads=[sgw[ws].name], writes=[wgb[ws].name])
                    P.add("pool", lambda e, ws=ws: e.tensor_copy(out=wub[ws][:], in_=suw[ws][:]), reads=[suw[ws].name], writes=[wub[ws].name])
                    P.add("act", lambda e, ws=ws: e.activation(out=wdb[ws][:], in_=sdw[ws][:], func=AF.Copy), reads=[sdw[ws].name], writes=[wdb[ws].name])
                    for (t0, nb_) in tiles:
                        W = nb_ * 128
                        at = actT[ti_ % 2]
                        ti_ += 1
                        for fs in range(2):
                            gu = pGU[gi_ % 4]
                            sg = sgt[gi_ % 4]
                            gi_ += 1

                            def gumm(e, gu=gu, ws=ws, fs=fs, t0=t0, W=W):
                                for which, wb in ((0, wgb[ws]), (1, wub[ws])):
                                    for k in range(8):
                                        ins = e.matmul(gu[:, which, 0:W], lhsT=wb[:, k, fs * 128:(fs + 1) * 128],
                                                       rhs=hnT[:, k, t0 * 128:t0 * 128 + W], start=(k == 0), stop=(k == 7))
                                return ins
                            P.add("pe", gumm, reads=[wgb[ws].name, wub[ws].name] + ["hnT%d" % (t0 + b) for b in range(nb_)], writes=[gu.name])
                            P.add("act", lambda e, gu=gu, sg=sg, W=W: e.activation(out=sg[:, 0:W], in_=gu[:, 0, 0:W], func=AF.Silu),
                                  reads=[gu.name], writes=[sg.name])
                            P.add("dve", lambda e, gu=gu, sg=sg, at=at, fs=fs, W=W: e.tensor_tensor(out=at[:, fs, 0:W], in0=sg[:, 0:W], in1=gu[:, 1, 0:W], op=ALU.mult),
                                  reads=[gu.name, sg.name], writes=[at.name + "_%d" % fs])

                        def emit_dn(at=at, ws=ws, t0=t0, nb_=nb_, ex_i=ex_i):
                            for b in range(nb_):
                                dn = pDN[b]
                                i = t0 + b

                                def dmm(e, dn=dn, b=b):
                                    for n in range(2):
                                        for fs in range(2):
                                            ins = e.matmul(dn[:, n * 512:(n + 1) * 512], lhsT=at[:, fs, b * 128:(b + 1) * 128],
                                                           rhs=wdb[ws][:, fs, n * 512:(n + 1) * 512], start=(fs == 0), stop=(fs == 1))
                                    return ins
                                P.add("pe", dmm, reads=[at.name + "_0", at.name + "_1", wdb[ws].name], writes=[dn.name])
                                if moe:
                                    P.add("dve", lambda e, dn=dn, i=i: e.scalar_tensor_tensor(out=acc[:, i, :], in0=dn[:], scalar=cw[:, i, ex_i:ex_i + 1],
                                                                                                in1=acc[:, i, :], op0=ALU.mult, op1=ALU.add),
                                          reads=[dn.name, "cw%d" % i, "acc%d" % i], writes=["acc%d" % i])
                                else:
                                    P.add("dve", lambda e, dn=dn, i=i: e.tensor_tensor(out=acc[:, i, :], in0=dn[:], in1=acc[:, i, :], op=ALU.add),
                                          reads=[dn.name, "acc%d" % i], writes=["acc%d" % i])
                        if pend[0] is not None:
                            pend[0]()
                        pend[0] = emit_dn
            if pend[0] is not None:
                pend[0]()
                pend[0] = None
            for i in range(G):
                jb = blk0 + i
                if L == 0:
                    P.dma("sp", H1[jb * 128:(jb + 1) * 128, :], acc[:, i, :], reads=["acc%d" % i], writes=["H1"], partial=True)
                else:
                    s = i % 2
                    ss, rs, ho = sss[s], rss[s], hb[s]
                    a_ap = acc[:, i, :]
                    ares = "acc%d" % i
                    P.add("act", lambda e, a_ap=a_ap, ss=ss: e.activation(out=junk[:], in_=a_ap, func=AF.Square, accum_out=ss[:]),
                          reads=[ares], writes=["junkD", ss.name])
                    P.add("act", lambda e, ss=ss, rs=rs: e.activation(out=rs[:], in_=ss[:], func=AF.Sqrt, bias=eps_t[:], scale=1.0 / D),
                          reads=[ss.name], writes=[rs.name])
                    P.add("dve", lambda e, rs=rs: e.reciprocal(out=rs[:], in_=rs[:]), reads=[rs.name], writes=[rs.name])
                    P.add("dve", lambda e, a_ap=a_ap, rs=rs, ho=ho: e.scalar_tensor_tensor(out=ho[:], in0=a_ap, scalar=rs[:, 0:1], in1=gf[:],
                                                                                         op0=ALU.mult, op1=ALU.mult),
                          reads=[ares, rs.name, "gfD"], writes=[ho.name])
                    P.dma("sp", out[jb * 128:(jb + 1) * 128, :], ho[:], reads=[ho.name], writes=["out"], partial=True)
            P.emit()
            blk0 += G

    plist = [("S", 0)] + [(ph, L) for L in range(2) for ph in "ABCD"]
    if phases is not None:
        plist = plist[:phases]
    for ph, L in plist:
        {"S": lambda L: phase_setup(), "A": phase_a, "B": phase_b, "C": phase_c, "D": phase_d}[ph](L)
    return nc


_IN_NAMES = ["rel_bias", "norm_mix", "w_in", "pool_group_w", "pool_scale", "lambda_q1", "lambda_k1", "lambda_q2",
             "lambda_k2", "subln_gain", "w_pool_up", "w_attn_up", "w_out", "norm_ffn", "dense_w_gate", "dense_w_up",
             "dense_w_down", "moe_router", "moe_w_gate", "moe_w_up", "moe_w_down"]


def run(inputs, dbg=False, phases=None):
    x = np.asarray(inputs["x"], np.float32)
    B, S, _ = x.shape
    meta = np.asarray(inputs["meta_tokens"], np.float32)
    Ltot = 16 + S
    NB = (Ltot + 127) // 128
    NI = ((NB + 3) // 4 + 1) // 2
    SL = [(i, qi) for i in range(NI) for qi in range(4) if 8 * i + qi < NB]
    NBH = len(SL)
    T = NB * 128
    ncores = 2 * B
    oh, invc = _const_tables()
    ident = np.eye(128, dtype=np.float32)
    shared = {k: np.ascontiguousarray(np.asarray(inputs[k], np.float32)) for k in _IN_NAMES}
    shared["final_norm"] = np.asarray(inputs["final_norm"], np.float32).reshape(1, D)
    shared["ident"] = ident
    shared["ohtab"] = oh
    shared["invcnt"] = invc
    in_maps = []
    for c in range(ncores):
        b, p = c // 2, c % 2
        xin = np.zeros((T, D), np.float32)
        xin[0:16] = meta
        xin[16:Ltot] = x[b]
        pv = np.zeros((128, 2), np.float32)
        pv[:, 0] = p
        pv[:, 1] = 1 - p
        m = dict(shared)
        m["xin"] = xin
        m["pvec"] = pv
        in_maps.append(m)
    nc = build(NB, NBH, dbg=dbg, phases=phases)
    res = run_bass_kernel_spmd(nc, in_maps, core_ids=list(range(ncores)))
    outs = []
    for b in range(B):
        full = np.empty((NB * 128, D), np.float32)
        for j in range(NB):
            p, k = (j // 4) % 2, SL.index((j // 8, j % 4))
            full[j * 128:(j + 1) * 128] = res.results[2 * b + p]["out"][k * 128:(k + 1) * 128]
        outs.append(full[16:Ltot])
    o = np.stack(outs, axis=0).astype(np.float32)
    if dbg:
        return o, res
    return o


def kernel(**inputs):
    return run(inputs)
```
